# Optimizing a Trainium2 kernel written in Bass

```python
import math
import jax, jax.numpy as jnp
from jax import lax
import numpy as np

D_MODEL = 1024
BATCH = 4
SEQ = 4096
DEPTH = 2

EPS = 1e-6
GLA_HEADS = 4
GLA_HDK = 48
GLA_HDV = 96
GLA_DK = GLA_HEADS * GLA_HDK
GLA_DV = GLA_HEADS * GLA_HDV
GLA_LR = 16
GLA_TAU = 16.0
GLA_CHUNK = 64
SB_HEADS = 6
SB_HD = 64
SB_W = SB_HEADS * SB_HD
SB_BLOCK = 128
POOL_WINDOWS = (2, 4, 8, 16)
POOL_GROUPS = 4
POOL_GC = 64
POOL_W = POOL_GROUPS * POOL_GC
POOL_MAX = 16
D_MIX = GLA_DV + SB_W + POOL_W
PROJ_SIZES = (GLA_DK, GLA_DK, GLA_DV, GLA_DV, GLA_LR, SB_W, SB_W, SB_W, POOL_W)
D_PROJ = GLA_DK * 2 + GLA_DV * 2 + GLA_LR + SB_W * 3 + POOL_W
MOE_GROUPS = 4
MOE_EPG = 8
N_EXPERTS = MOE_GROUPS * MOE_EPG
MOE_TOPK = 2
D_EXPERT = 256
PLE_DIM = 256

kernel_name = "hybrid_gla_stickbreak_pool_hmoe"


def rmsnorm(x, g):
    xf = x.astype(jnp.float32)
    y = xf * lax.rsqrt(jnp.mean(xf * xf, axis=-1, keepdims=True) + EPS)
    return (y * g.astype(jnp.float32)).astype(x.dtype)


def gla_mixer(q, k, v, g_out, lr, w_lr2, b_lr, norm_g):
    B, S, _ = q.shape
    n = S // GLA_CHUNK
    f32 = jnp.float32
    log_a = jax.nn.log_sigmoid((lr @ w_lr2 + b_lr).astype(f32)) / GLA_TAU

    def heads(t, dh):
        return t.reshape(B, n, GLA_CHUNK, GLA_HEADS, dh).transpose(0, 3, 1, 2, 4).astype(f32)

    qh = heads(q, GLA_HDK) * (GLA_HDK ** -0.5)
    kh = heads(k, GLA_HDK)
    vh = heads(v, GLA_HDV)
    b = jnp.cumsum(heads(log_a, GLA_HDK), axis=3)
    b_last = b[:, :, :, -1:, :]
    q_dec = qh * jnp.exp(b)
    k_in = kh * jnp.exp(-b)
    k_out = kh * jnp.exp(b_last - b)
    causal = jnp.tril(jnp.ones((GLA_CHUNK, GLA_CHUNK), dtype=bool))
    att = jnp.where(causal, jnp.einsum('bhncd,bhnsd->bhncs', q_dec, k_in), 0.0)
    o_intra = jnp.einsum('bhncs,bhnsv->bhncv', att, vh)
    kv = jnp.einsum('bhncd,bhncv->bhndv', k_out, vh)
    decay = jnp.exp(b_last[:, :, :, 0, :])

    def step(state, inp):
        kv_n, d_n = inp
        return d_n[..., None] * state + kv_n, state

    s0 = jnp.zeros((B, GLA_HEADS, GLA_HDK, GLA_HDV), f32)
    _, states = lax.scan(step, s0, (jnp.moveaxis(kv, 2, 0), jnp.moveaxis(decay, 2, 0)))
    states = jnp.moveaxis(states, 0, 2)
    o = o_intra + jnp.einsum('bhncd,bhndv->bhncv', q_dec, states)
    o = rmsnorm(o, norm_g)
    o = o.transpose(0, 2, 3, 1, 4).reshape(B, S, GLA_DV)
    return (o * jax.nn.silu(g_out.astype(f32))).astype(v.dtype)


def stick_breaking(q, k, v):
    B, S, _ = q.shape
    f32 = jnp.float32

    def heads(t):
        return t.reshape(B, S, SB_HEADS, SB_HD).transpose(0, 2, 1, 3).astype(f32)

    qh = heads(q) * (SB_HD ** -0.5)
    kh = heads(k)
    vh = heads(v)
    outs = []
    for blk in range(S // SB_BLOCK):
        q0 = blk * SB_BLOCK
        end = q0 + SB_BLOCK
        z = jnp.einsum('bhqd,bhkd->bhqk', qh[:, :, q0:end], kh[:, :, :end])
        qpos = q0 + jnp.arange(SB_BLOCK)[:, None]
        kpos = jnp.arange(end)[None, :]
        mask = kpos < qpos
        log_1m = jnp.where(mask, jax.nn.log_sigmoid(-z), 0.0)
        suffix = lax.cumsum(log_1m, axis=3, reverse=True) - log_1m
        a = jnp.where(mask, jnp.exp(jax.nn.log_sigmoid(z) + suffix), 0.0)
        outs.append(jnp.einsum('bhqk,bhkd->bhqd', a, vh[:, :, :end]))
    o = jnp.concatenate(outs, axis=2)
    return o.transpose(0, 2, 1, 3).reshape(B, S, SB_W).astype(v.dtype)


def pool_mixer(u, w_pool, scale):
    B, S, _ = u.shape
    uf = u.astype(jnp.float32)
    cs = jnp.pad(jnp.cumsum(uf, axis=1), ((0, 0), (POOL_MAX, 0), (0, 0)))
    pos = jnp.arange(S, dtype=jnp.float32)[None, :, None]
    groups = []
    for gi, w in enumerate(POOL_WINDOWS):
        c = slice(gi * POOL_GC, (gi + 1) * POOL_GC)
        wsum = cs[:, POOL_MAX:POOL_MAX + S, c] - cs[:, POOL_MAX - w:POOL_MAX - w + S, c]
        cnt = jnp.minimum(pos + 1.0, float(w))
        groups.append(wsum / cnt - uf[:, :, c])
    pooled = jnp.stack(groups, axis=2)
    y = jnp.einsum('bsgc,gcd->bsgd', pooled, w_pool.astype(jnp.float32)).reshape(B, S, POOL_W)
    return (y * scale.astype(jnp.float32)).astype(u.dtype)


def hier_moe(h, wg, bg, we, be, w_gate, w_up, w_down):
    B, S, D = h.shape
    t = h.reshape(B * S, D)
    f32 = jnp.float32
    g_prob = jax.nn.softmax((t @ wg + bg).astype(f32), axis=-1)
    g_w, g_idx = lax.top_k(g_prob, 1)
    g_onehot = jax.nn.one_hot(g_idx[:, 0], MOE_GROUPS, dtype=f32)
    e_logits = (t @ we + be).astype(f32).reshape(-1, MOE_GROUPS, MOE_EPG)
    e_in = jnp.einsum('tge,tg->te', e_logits, g_onehot)
    e_w, e_idx = lax.top_k(jax.nn.softmax(e_in, axis=-1), MOE_TOPK)
    e_w = e_w / jnp.sum(e_w, axis=-1, keepdims=True) * g_w
    expert_id = g_idx * MOE_EPG + e_idx
    combine = jnp.einsum('tk,tke->te', e_w, jax.nn.one_hot(expert_id, N_EXPERTS, dtype=f32))
    y = jnp.zeros((B * S, D), f32)
    for gi in range(MOE_GROUPS):
        sl = slice(gi * MOE_EPG, (gi + 1) * MOE_EPG)
        a = jnp.einsum('td,edf->tef', t, w_gate[sl])
        b = jnp.einsum('td,edf->tef', t, w_up[sl])
        hid = jax.nn.silu(a.astype(f32)) * b.astype(f32) * combine[:, sl, None]
        y = y + jnp.einsum('tef,efd->td', hid, w_down[sl].astype(f32))
    return y.reshape(B, S, D).astype(h.dtype)


def setup_inputs(seed: int = 0) -> dict:
    key = jax.random.key(seed)
    ks = jax.random.split(key, 24)
    L, D = DEPTH, D_MODEL
    nrm = lambda k, shape, s: jax.random.normal(k, shape, jnp.float32) * s
    gain = lambda k, shape: 1.0 + 0.02 * jax.random.normal(k, shape, jnp.float32)
    return {
        "x": nrm(ks[0], (BATCH, SEQ, D), 1.0),
        "p": nrm(ks[1], (DEPTH, BATCH, SEQ, PLE_DIM), 1.0),
        "norm1_g": gain(ks[2], (L, D)),
        "w_in": nrm(ks[3], (L, D, D_PROJ), D ** -0.5),
        "gla_w_lr2": nrm(ks[4], (L, GLA_LR, GLA_DK), GLA_LR ** -0.5),
        "gla_b_lr": nrm(ks[5], (L, GLA_DK), 0.1),
        "gla_norm_g": gain(ks[6], (L, GLA_HDV)),
        "pool_w": nrm(ks[7], (L, POOL_GROUPS, POOL_GC, POOL_GC), POOL_GC ** -0.5),
        "pool_scale": gain(ks[8], (L, POOL_W)),
        "w_out": nrm(ks[9], (L, D_MIX, D), D_MIX ** -0.5),
        "norm2_g": gain(ks[10], (L, D)),
        "router_group_w": nrm(ks[11], (L, D, MOE_GROUPS), D ** -0.5),
        "router_group_b": nrm(ks[12], (L, MOE_GROUPS), 0.01),
        "router_exp_w": nrm(ks[13], (L, D, N_EXPERTS), D ** -0.5),
        "router_exp_b": nrm(ks[14], (L, N_EXPERTS), 0.01),
        "exp_w_gate": nrm(ks[15], (L, N_EXPERTS, D, D_EXPERT), D ** -0.5),
        "exp_w_up": nrm(ks[16], (L, N_EXPERTS, D, D_EXPERT), D ** -0.5),
        "exp_w_down": nrm(ks[17], (L, N_EXPERTS, D_EXPERT, D), D_EXPERT ** -0.5),
        "ple_norm_g": gain(ks[18], (L, D)),
        "ple_gate_w": nrm(ks[19], (L, D, D), D ** -0.5),
        "ple_gate_b": nrm(ks[20], (L, D), 0.01),
        "ple_proj_w": nrm(ks[21], (L, PLE_DIM, D), PLE_DIM ** -0.5),
        "final_norm_g": gain(ks[22], (D,)),
    }


def reference(x, p, norm1_g, w_in, gla_w_lr2, gla_b_lr, gla_norm_g, pool_w, pool_scale, w_out,
              norm2_g, router_group_w, router_group_b, router_exp_w, router_exp_b,
              exp_w_gate, exp_w_up, exp_w_down, ple_norm_g, ple_gate_w, ple_gate_b, ple_proj_w,
              final_norm_g):
    h = x
    for i in range(DEPTH):
        hn = rmsnorm(h, norm1_g[i])
        proj = hn @ w_in[i]
        parts = []
        off = 0
        for sz in PROJ_SIZES:
            parts.append(proj[..., off:off + sz])
            off += sz
        g_q, g_k, g_v, g_o, g_lr, s_q, s_k, s_v, pool_u = parts
        o_gla = gla_mixer(g_q, g_k, g_v, g_o, g_lr, gla_w_lr2[i], gla_b_lr[i], gla_norm_g[i])
        o_sb = stick_breaking(s_q, s_k, s_v)
        o_pool = pool_mixer(pool_u, pool_w[i], pool_scale[i])
        mix = jnp.concatenate([o_gla, o_sb, o_pool], axis=-1)
        h = h + mix @ w_out[i]
        h = h + hier_moe(rmsnorm(h, norm2_g[i]), router_group_w[i], router_group_b[i],
                         router_exp_w[i], router_exp_b[i], exp_w_gate[i], exp_w_up[i], exp_w_down[i])
        gate = jax.nn.sigmoid((rmsnorm(h, ple_norm_g[i]) @ ple_gate_w[i] + ple_gate_b[i]).astype(jnp.float32))
        e = (p[i] @ ple_proj_w[i]).astype(jnp.float32)
        h = h + (gate * e).astype(h.dtype)
    return rmsnorm(h, final_norm_g)
```

```python
import numpy as np
import ml_dtypes
from contextlib import ExitStack
import concourse.bass as bass
import concourse.mybir as mybir
from concourse.bass_utils import run_bass_kernel_spmd
from concourse.alu_op_type import AluOpType as ALU

AF = mybir.ActivationFunctionType
AX = mybir.AxisListType
F32 = mybir.dt.float32
BF16 = mybir.dt.bfloat16
NPBF = ml_dtypes.bfloat16

D = 1024
T = 2048
NT = 16
NG = 4
DEPTH = 2
EPS = 1e-6
NEXP = 32
O_GQ, O_GK, O_GV, O_GO, O_LR, O_SQ, O_SK, O_SV, O_PU = 0, 192, 384, 768, 1152, 1168, 1552, 1936, 2320


class Reg:
    __slots__ = ("name", "writer", "readers", "dsem", "dcount", "lastdma", "maxw", "rt")

    def __init__(self, name):
        self.name = name
        self.writer = None
        self.readers = []
        self.dsem = None
        self.dcount = 0
        self.lastdma = None
        self.maxw = 0
        self.rt = None


class Op:
    __slots__ = ("eng", "fn", "deps", "signal", "is_dma", "dreg", "dval", "rank", "idx")


class Prog:
    ENG = ("pe", "act", "dve", "pool", "sp")

    def __init__(self, nc):
        self.nc = nc
        self.ops = {e: [] for e in self.ENG}
        self.n = 0
        self.dma_regs = []
        self.all_regs = []
        self.fence_deps = []
        self.fence_pending = set()
        self.regcache = {}

    def reg(self, name):
        if name in self.regcache:
            return self.regcache[name]
        r = Reg(name)
        self.regcache[name] = r
        self.all_regs.append(r)
        return r

    def regs(self, name, n):
        return [self.reg(f"{name}{i}") for i in range(n)]

    def fence(self):
        deps = []
        for e in self.ENG:
            for o in reversed(self.ops[e]):
                if not o.is_dma:
                    deps.append(o)
                    break
        for r in self.dma_regs:
            if r.lastdma is not None:
                deps.append(r.lastdma)
        for d in deps:
            d.signal = True
        self.fence_deps = deps
        self.fence_pending = set(self.ENG)
        for r in self.all_regs:
            r.writer = None
            r.readers = []

    def op(self, eng, fn, reads=(), writes=(), dma=False, accum=False):
        o = Op()
        o.eng = eng
        o.fn = fn
        o.is_dma = dma
        o.signal = False
        o.dreg = None
        o.dval = 0
        o.rank = 0
        o.idx = self.n
        self.n += 1
        deps = []
        if eng in self.fence_pending:
            self.fence_pending.discard(eng)
            deps.extend(self.fence_deps)
        for r in reads:
            if r.writer is not None:
                deps.append(r.writer)
        for r in writes:
            w = r.writer
            if w is not None:
                if accum and w.eng == eng and not w.is_dma:
                    pass
                elif dma and w.is_dma:
                    pass
                else:
                    deps.append(w)
            deps.extend(r.readers)
        seen = set()
        dl = []
        for d in deps:
            if d.idx in seen:
                continue
            seen.add(d.idx)
            if d.is_dma:
                val = 16 * d.dreg.dcount
                d.dreg.maxw = max(d.dreg.maxw, val)
                dl.append((d, val))
            else:
                dl.append((d, None))
                d.signal = True
        o.deps = dl
        if dma:
            r = writes[0]
            if r.dsem is None:
                self.dma_regs.append(r)
                r.dsem = True
            if r.maxw > 0:
                dl.append((r.lastdma, r.maxw))
            r.dcount += 1
            r.lastdma = o
            o.dreg = r
            o.dval = 16 * r.dcount
        for r in reads:
            r.readers.append(o)
        for r in writes:
            r.writer = o
            r.readers = []
        self.ops[eng].append(o)
        return o

    def emit(self, outputs_final=()):
        nc = self.nc
        with ExitStack() as es:
            esem = {e: es.enter_context(nc.semaphore(f"s_{e}")) for e in self.ENG}
            for i, r in enumerate(self.dma_regs):
                r.dsem = es.enter_context(nc.semaphore(f"d{i}_{r.name}"))
            for e in self.ENG:
                k = 0
                for o in self.ops[e]:
                    if o.is_dma:
                        continue
                    if o.signal:
                        k += 1
                        o.rank = k
            block = es.enter_context(nc.Block())

            def run(ename, eng):
                waited = {}
                for o in self.ops[ename]:
                    for d, dv in o.deps:
                        if d.is_dma:
                            sem, val = d.dreg.dsem, dv
                        else:
                            sem, val = esem[d.eng], d.rank
                        key = id(sem)
                        if waited.get(key, 0) >= val:
                            continue
                        waited[key] = val
                        eng.wait_ge(sem, val)
                    ins = o.fn(eng)
                    if o.is_dma:
                        ins.then_inc(o.dreg.dsem, 16)
                    elif o.signal:
                        ins.then_inc(esem[ename], 1)
                if ename == "sp":
                    for r in outputs_final:
                        if r.dcount:
                            eng.wait_ge(r.dsem, 16 * r.dcount)

            block.tensor(lambda eng: run("pe", eng))
            block.scalar(lambda eng: run("act", eng))
            block.vector(lambda eng: run("dve", eng))
            block.gpsimd(lambda eng: run("pool", eng))
            block.sync(lambda eng: run("sp", eng))


ARENA_BYTES = 202 * 1024


class Arena:
    def __init__(self, ap_bf16, P):
        self.ap = ap_bf16
        self.off = 0
        self.P = P
        self.hi = 0

    def alloc(self, name, free_shape, dt, reg=True):
        esz = 4 if dt == F32 else 2
        n = int(np.prod(free_shape))
        nbytes = (n * esz + 31) // 32 * 32
        assert self.off + nbytes <= ARENA_BYTES, (name, self.off, nbytes)
        v = self.ap[:, self.off // 2:(self.off + n * esz) // 2]
        if dt == F32:
            v = v.bitcast(F32)
        if len(free_shape) == 2:
            v = v.rearrange("p (a b) -> p a b", a=free_shape[0])
        elif len(free_shape) == 3:
            v = v.rearrange("p (a b c) -> p a b c", a=free_shape[0], b=free_shape[1])
        self.off += nbytes
        self.hi = max(self.hi, self.off)
        return (v, self.P.reg(name)) if reg else v

    def mark(self):
        return self.off

    def release(self, m):
        self.off = m


def build(L, stages=99):
    nc = bass.Bass("TRN2", target_bir_lowering=False)
    P = Prog(nc)

    def din(name, shape, dt=F32):
        return nc.dram_tensor(name, list(shape), dt, kind="ExternalInput").ap()

    def dout(name, shape, dt=F32):
        return nc.dram_tensor(name, list(shape), dt, kind="ExternalOutput").ap()

    h0 = din("h0a", [T, D]); h0b = din("h0b", [T, D])
    p_a = din("pa", [L, T, 256]); p_b = din("pb", [L, T, 256])
    ng = din("ng", [L, 3, D]); fg = din("fg", [D])
    w_in = din("w_in", [L, D, 2576]); w_lr2 = din("w_lr2", [L, 16, 192]); b_lr = din("b_lr", [L, 192])
    gng = din("gng", [L, 96]); pool_w = din("pool_w", [L, 4, 64, 64]); pool_s = din("pool_s", [L, 256])
    w_out = din("w_out", [L, D, D]); wr = din("wr", [L, D, 36]); br = din("br", [L, 36])
    wg = din("wg", [L, NEXP, D, 256]); wu = din("wu", [L, NEXP, D, 256]); wd = din("wd", [L, NEXP, 256, D])
    pgw = din("pgw", [L, D, D]); pgb = din("pgb", [L, D]); ppw = din("ppw", [L, 256, D])
    c_ident = din("c_ident", [128, 128], BF16); c_tri = din("c_tri", [128, 128], BF16)
    c_ones = din("c_ones", [128, 128], BF16)
    c_sbmask = din("c_sbmask", [128, 512], BF16); c_glamask = din("c_glamask", [128, 512], BF16)
    c_scanmask = din("c_scanmask", [128, 512], BF16); c_evenmask = din("c_evenmask", [128, 512], BF16)
    c_oddmask = din("c_oddmask", [128, 512], BF16)
    c_invw = din("c_invw", [128, 2])
    r_prevbias = din("r_prevbias", [128, 1]); r_flag = din("r_flag", [128, 1]); r_invcnt = din("r_invcnt", [128, 2, 16])
    c_invcnt0 = din("c_invcnt0", [128, 2, 16])

    def dscr(name, shape, dt=F32):
        return nc.dram_tensor(name, list(shape), dt).ap()

    xk = dscr("ex_k", [L, 128, 3, T], BF16); xv = dscr("ex_v", [L, 128, 16, 384], BF16)
    xs = dscr("ex_s", [L, 128, 2, 96]); xu = dscr("ex_u", [L, 128, 2, 16])
    ok, ov, osd, oud = xk, xv, xs, xu
    hsA = dscr("hsA", [T, D]); hsB = dscr("hsB", [T, D])
    y_out = dout("y_out", [T, D])
    r_hsA = P.reg("hsA"); r_hsB = P.reg("hsB")
    r_yout = P.reg("y_out"); r_ok = P.reg("ok"); r_ov = P.reg("ov")
    r_os = P.reg("os"); r_ou = P.reg("ou")

    es = ExitStack()
    arena_t = es.enter_context(nc.sbuf_tensor("arena", [128, ARENA_BYTES // 2], BF16))
    A = Arena(arena_t[:], P)
    pbank = [es.enter_context(nc.psum_tensor(f"pb{i}", [128, 512], F32)) for i in range(6)]
    pbank += [es.enter_context(nc.psum_tensor(f"pb{i}", [128, 1024], BF16)) for i in (6, 7)]
    rpb = P.regs("pb", 8)

    def pbf(i):
        assert i < 6
        return pbank[i][:]

    def pbb(i):
        assert i >= 6
        return pbank[i][:]

    def MM(out, lhsT, rhs, start, stop, rd, wr):
        rt = (lhsT.base_partition(), lhsT.partition_size())
        acc = all(r.rt is None or r.rt == rt for r in wr)
        P.op("pe", lambda e: e.matmul(out, lhsT=lhsT, rhs=rhs, start=start, stop=stop), rd, wr, accum=acc)
        for r in wr:
            r.rt = rt

    def TR(out, in_, rd, wr):
        rt = (0, 128)
        acc = all(r.rt is None or r.rt == rt for r in wr)
        P.op("pe", lambda e: e.transpose(out=out, in_=in_, identity=ident), rd + [r_const], wr, accum=acc)
        for r in wr:
            r.rt = rt

    def ACT(out, in_, func, rd, wr, **kw):
        P.op("act", lambda e: e.activation(out=out, in_=in_, func=func, **kw), rd, wr)

    def TT(eng, out, in0, in1, op, rd, wr):
        P.op(eng, lambda e: e.tensor_tensor(out=out, in0=in0, in1=in1, op=op), rd, wr)

    def TS(eng, out, in0, s1, s2, op0, op1, rd, wr):
        if op1 is None:
            P.op(eng, lambda e: e.tensor_scalar(out=out, in0=in0, scalar1=s1, scalar2=None, op0=op0), rd, wr)
        else:
            P.op(eng, lambda e: e.tensor_scalar(out=out, in0=in0, scalar1=s1, scalar2=s2, op0=op0, op1=op1), rd, wr)

    def STT(out, in0, scalar, in1, op0, op1, rd, wr):
        P.op("dve", lambda e: e.scalar_tensor_tensor(out=out, in0=in0, scalar=scalar, in1=in1, op0=op0, op1=op1), rd, wr)

    def CP(eng, out, in_, rd, wr):
        if eng == "act":
            ACT(out, in_, AF.Copy, rd, wr)
        else:
            P.op(eng, lambda e: e.tensor_copy(out=out, in_=in_), rd, wr)

    def MSET(eng, ap, val, wr):
        P.op(eng, lambda e: e.memset(ap, val), [], wr)

    def DMA(eng, out, in_, rd, wr):
        P.op(eng, lambda e: e.dma_start(out=out, in_=in_), rd, wr, dma=True)

    def RED(out, in_, op, rd, wr):
        P.op("dve", lambda e: e.tensor_reduce(out=out, in_=in_, axis=AX.X, op=op), rd, wr)

    def RECIP(out, in_, rd, wr):
        P.op("dve", lambda e: e.reciprocal(out=out, in_=in_), rd, wr)

    KC = "(c p) n -> p c n"

    h, _ = A.alloc("h", [NT, D], F32)
    rh = P.regs("h", NT)
    r_hload = P.reg("hload")
    r_const = P.reg("const")
    ident = A.alloc("ident", [128], BF16, reg=False)
    tri = A.alloc("tri", [128], BF16, reg=False)
    ones = A.alloc("ones", [128], BF16, reg=False)
    sbmask = A.alloc("sbmask", [512], BF16, reg=False)
    invw = A.alloc("invw", [2], F32, reg=False)
    prevbias = A.alloc("prevbias", [1], F32, reg=False)
    flag = A.alloc("flag", [1], F32, reg=False)
    invcnt = A.alloc("invcnt", [2, 16], F32, reg=False)
    invcnt0 = A.alloc("invcnt0", [2, 16], F32, reg=False)
    for dst, src in ((ident, c_ident), (tri, c_tri), (ones, c_ones), (sbmask, c_sbmask), (invw, c_invw),
                     (prevbias, r_prevbias), (flag, r_flag), (invcnt, r_invcnt),
                     (invcnt0, c_invcnt0)):
        DMA("sp", dst, src, [], [r_const])
    for q in range(4):
        DMA("sp", h[:, 4 * q:4 * q + 4, :], h0[512 * q:512 * q + 512, :].rearrange("(t p) d -> p t d", p=128),
            [], [r_hload] + rh[4 * q:4 * q + 4])

    actT, _ = A.alloc("actT", [8, T], BF16)
    ractT = P.regs("actT", NG)

    def norm_to_T(gsrc_ap):
        gbc, r_g = A.alloc("gbc", [D], F32)
        junk, r_junk = A.alloc("junk", [D], BF16)
        ssq, r_ss = A.alloc("ssq", [NT], F32)
        rstd, r_rstd = A.alloc("rstd", [NT], F32)
        xn = [A.alloc(f"xn{i}", [D], BF16) for i in range(2)]
        DMA("sp", gbc, gsrc_ap.partition_broadcast(128), [], [r_g])
        for t in range(NT):
            ACT(junk, h[:, t, :], AF.Square, [rh[t]], [r_junk, r_ss], accum_out=ssq[:, t:t + 1])
        ACT(rstd, ssq, AF.Sqrt, [r_ss], [r_rstd], scale=1.0 / D, bias=EPS)
        RECIP(rstd, rstd, [r_rstd], [r_rstd])
        for t in range(NT):
            xb, r_xb = xn[t % 2]
            STT(xb, h[:, t, :], rstd[:, t:t + 1], gbc, ALU.mult, ALU.mult, [rh[t], r_rstd, r_g], [r_xb])
            pi = 6 + t % 2
            pv = pbb(pi).rearrange("p (c k) -> p c k", c=8)
            for c in range(8):
                TR(pv[:, c, :], xb[:, 128 * c:128 * c + 128], [r_xb], [rpb[pi]])
            CP("act" if t % 2 == 0 else "dve", actT[:, :, 128 * t:128 * t + 128], pv, [rpb[pi]], [ractT[t // 4]])

    def add_to_h(t, half, ps_ap, rps):
        hs = h[:, t, 512 * half:512 * half + 512]
        TT("dve", hs, hs, ps_ap, ALU.add, [rh[t], rps], [rh[t]])

    class _Stop(Exception):
        pass

    class _SegEnd(Exception):
        pass

    def swap_h(store_ap, r_store, load_ap, r_load):
        if store_ap is not None:
            for q in range(4):
                DMA("sp", store_ap[512 * q:512 * q + 512, :].rearrange("(t p) d -> p t d", p=128), h[:, 4 * q:4 * q + 4, :],
                    rh[4 * q:4 * q + 4], [r_store])
        for q in range(4):
            DMA("pool", h[:, 4 * q:4 * q + 4, :], load_ap[512 * q:512 * q + 512, :].rearrange("(t p) d -> p t d", p=128),
                [r_load] if r_load is not None else [], [r_hload] + rh[4 * q:4 * q + 4])

    def stage_gate(x):
        if stages < x:
            raise _Stop()

    r_h0b = None
    segs = [(l, sg) for l in range(L) for sg in (0, 1)]
    stopped = False
    for (l, sg) in segs:
      if stopped:
          break
      X0 = (sg == 0)
      partial = X0 and (l == L - 1)
      p_in = p_a if X0 else p_b
      if (l, sg) == (0, 1):
          swap_h(hsA, r_hsA, h0b, None)
      elif sg == 0 and l > 0:
          swap_h(hsB, r_hsB, hsA, r_hsA)
      elif sg == 1 and l > 0:
          swap_h(None, None, hsB, r_hsB)
      P.fence()
      m_layer = A.mark()
      try:
          m = A.mark()
          if stages >= 1:
              norm_to_T(ng[l, 0])
          P.fence(); A.release(m)
          if stages <= 1:
              raise _Stop()
          m = A.mark()
          NC2 = 1552
          C_Q, C_K, C_V, C_G, C_LR, C_PU = 0, 256, 512, 896, 1280, 1296
          wgp, r_wgp = A.alloc("wgp", [8, NC2], BF16)
          wout_gp, r_wout_gp = A.alloc("wout_gp", [5, D], BF16)
          wlr2p, r_wl = A.alloc("wlr2p", [256], BF16)
          negb, r_negb = A.alloc("negb", [2], F32)
          gngbc, r_gng = A.alloc("gngbc", [4, 96], F32)
          poolbd, r_poolbd = A.alloc("poolbd", [2, 128], BF16)
          pscol, r_pscol = A.alloc("pscol", [2], F32)
          glamask = A.alloc("glamask", [512], BF16, reg=False)
          scanmask = A.alloc("scanmask", [512], BF16, reg=False)
          evenmask = A.alloc("evenmask", [512], BF16, reg=False)
          oddmask = A.alloc("oddmask", [512], BF16, reg=False)
          r_c2 = P.reg("c2")
          for dst, src in ((glamask, c_glamask), (scanmask, c_scanmask), (evenmask, c_evenmask), (oddmask, c_oddmask)):
              DMA("sp", dst, src, [], [r_c2])
          MSET("pool", wgp[:, :, 0:512], 0.0, [r_wgp])
          MSET("pool", wlr2p, 0.0, [r_wl])
          MSET("pool", negb, 0.0, [r_negb])
          MSET("pool", poolbd, 0.0, [r_poolbd])
          wl = w_in[l]
          for hh in range(4):
              DMA("pool", wgp[:, :, C_Q + 64 * hh:C_Q + 64 * hh + 48],
                  wl[:, O_GQ + 48 * hh:O_GQ + 48 * hh + 48].rearrange(KC, p=128), [], [r_wgp])
              DMA("pool", wgp[:, :, C_K + 64 * hh:C_K + 64 * hh + 48],
                  wl[:, O_GK + 48 * hh:O_GK + 48 * hh + 48].rearrange(KC, p=128), [], [r_wgp])
              DMA("pool", wlr2p[0:16, 64 * hh:64 * hh + 48], w_lr2[l, :, 48 * hh:48 * hh + 48], [], [r_wl])
              pr, sl = hh // 2, 64 * (hh % 2)
              DMA("sp", negb[sl:sl + 48, pr:pr + 1], b_lr[l, 48 * hh:48 * hh + 48].rearrange("(p o) -> p o", o=1), [], [r_negb])
              DMA("sp", gngbc[:, hh, :], gng[l].partition_broadcast(128), [], [r_gng])
              DMA("pool", poolbd[sl:sl + 64, pr, sl:sl + 64], pool_w[l, hh], [], [r_poolbd])
          DMA("pool", wgp[:, :, C_V:C_V + 768], wl[:, O_GV:O_GV + 768].rearrange(KC, p=128), [], [r_wgp])
          DMA("pool", wgp[:, :, C_LR:C_LR + 16], wl[:, O_LR:O_LR + 16].rearrange(KC, p=128), [], [r_wgp])
          DMA("pool", wgp[:, :, C_PU:C_PU + 256], wl[:, O_PU:O_PU + 256].rearrange(KC, p=128), [], [r_wgp])
          DMA("pool", wout_gp[:, 0:3, :], w_out[l, 0:384, :].rearrange(KC, p=128), [], [r_wout_gp])
          DMA("pool", wout_gp[:, 3:5, :], w_out[l, 768:1024, :].rearrange(KC, p=128), [], [r_wout_gp])
          for j in range(2):
              DMA("sp", pscol[:, j:j + 1], pool_s[l, 128 * j:128 * j + 128].rearrange("(p o) -> p o", o=1), [], [r_pscol])
          TS("dve", negb, negb, -1.0, None, ALU.mult, None, [r_negb], [r_negb])
          stage_gate(1.2)

          S32, r_S32 = A.alloc("S32", [2, 96], F32)
          Sbf = [A.alloc(f"Sbf{i}", [2, 96], BF16) for i in range(2)]
          stmp, r_stmp = A.alloc("stmp", [2, 96], F32)
          if X0:
              MSET("dve", S32, 0.0, [r_S32])
          else:
              DMA("sp", S32, xs[l], [r_os], [r_S32])
              TS("dve", S32, S32, flag[:, 0:1], None, ALU.mult, None, [r_S32, r_const], [r_S32])
          CP("dve", Sbf[0][0], S32, [r_S32], [Sbf[0][1]])
          sb_i = 0
          ubuf, r_ubuf = A.alloc("ubuf", [2, 528], F32)
          if X0:
              MSET("dve", ubuf[:, :, 0:16], 0.0, [r_ubuf])
          else:
              DMA("sp", ubuf[:, :, 0:16], xu[l], [r_ou], [r_ubuf])
              TS("dve", ubuf[:, :, 0:16], ubuf[:, :, 0:16], flag[:, 0:1], None, ALU.mult, None, [r_ubuf, r_const], [r_ubuf])
          s2, r_s2 = A.alloc("s2", [2, 528], F32)
          s4, r_s4 = A.alloc("s4", [2, 528], F32)
          s8, r_s8 = A.alloc("s8", [528], F32)
          s16, r_s16 = A.alloc("s16", [528], F32)
          pooled, r_pooled = A.alloc("pooled", [2, 512], BF16)
          ptmp, r_ptmp = A.alloc("ptmp", [2, 16], F32)
          lrT, r_lrT = A.alloc("lrT", [512], BF16)
          ta, r_ta = A.alloc("ta", [512], F32)
          cbt, r_cb = A.alloc("cb", [512], F32)
          enb, r_enb = A.alloc("enb", [512], F32)
          eb = [A.alloc(f"eb{j}", [512], F32) for j in range(2)]
          qd, r_qd = A.alloc("qd", [512], BF16)
          qde = [A.alloc(f"qde{j}", [512], BF16) for j in range(2)]
          qdo = [A.alloc(f"qdo{j}", [512], BF16) for j in range(2)]
          kin = [A.alloc(f"kin{j}", [512], BF16) for j in range(2)]
          kintok, r_kintok = A.alloc("kintok", [4, 2, 128], BF16)
          vtok, r_vtok = A.alloc("vtok", [4, 384], BF16)
          gsil, r_gsil = A.alloc("gsil", [4, 384], BF16)
          attm, r_attm = A.alloc("attm", [4, 128], BF16)
          osb, r_osb = A.alloc("osb", [4, 96], F32)
          osq, r_osq = A.alloc("osq", [4, 96], F32)
          oss, r_oss = A.alloc("oss", [4], F32)
          orstd, r_orstd = A.alloc("orstd", [4], F32)
          ogla, r_ogla = A.alloc("ogla", [384], BF16)
          mixgp, r_mixgp = A.alloc("mixgp", [5, 512], BF16)

          for g in range(NG):
              gs = slice(512 * g, 512 * g + 512)
              rA = [ractT[g]]
              for c in range(8):
                  MM(pbf(0)[0:16, :], wgp[:, c, C_LR:C_LR + 16], actT[:, c, gs], c == 0, c == 7, rA + [r_wgp], [rpb[0]])
              CP("act", lrT[0:16, :], pbf(0)[0:16, :], [rpb[0]], [r_lrT])
              stage_gate(1.3)
              for j in range(2):
                  MM(pbf(1), wlr2p[0:16, 128 * j:128 * j + 128], lrT[0:16, :], True, True, [r_wl, r_lrT], [rpb[1]])
                  ACT(ta, pbf(1), AF.Exp, [rpb[1], r_negb], [r_ta], scale=-1.0, bias=negb[:, j:j + 1])
                  ACT(ta, ta, AF.Ln, [r_ta], [r_ta], bias=1.0)
                  P.op("dve", lambda e: e.tensor_tensor_scan(out=cbt, data0=scanmask, data1=ta, initial=0.0,
                                                            op0=ALU.mult, op1=ALU.add), [r_ta, r_c2], [r_cb])
                  ACT(eb[j][0], cbt, AF.Exp, [r_cb], [eb[j][1]], scale=-1.0 / 16)
                  ACT(enb, cbt, AF.Exp, [r_cb], [r_enb], scale=1.0 / 16)
                  if not partial:
                      for c in range(8):
                          MM(pbf(3), wgp[:, c, C_Q + 128 * j:C_Q + 128 * j + 128], actT[:, c, gs], c == 0, c == 7, rA + [r_wgp], [rpb[3]])
                      STT(qd, pbf(3), 48 ** -0.5, eb[j][0], ALU.mult, ALU.mult, [rpb[3], eb[j][1]], [r_qd])
                      TT("pool", qde[j][0], qd, evenmask, ALU.mult, [r_qd, r_c2], [qde[j][1]])
                      TT("pool", qdo[j][0], qd, oddmask, ALU.mult, [r_qd, r_c2], [qdo[j][1]])
                  for c in range(8):
                      MM(pbf(4), wgp[:, c, C_K + 128 * j:C_K + 128 * j + 128], actT[:, c, gs], c == 0, c == 7, rA + [r_wgp], [rpb[4]])
                  TT("dve", kin[j][0], pbf(4), enb, ALU.mult, [rpb[4], r_enb], [kin[j][1]])
              stage_gate(1.4)
              kv_ps = pbb(6).rearrange("p (t j k) -> p t j k", t=4, j=2)
              for tt in range(4):
                  for j in range(2):
                      TR(kv_ps[:, tt, j, :], kin[j][0][:, 128 * tt:128 * tt + 128], [kin[j][1]], [rpb[6]])
              CP("act", kintok, kv_ps, [rpb[6]], [r_kintok])
              stage_gate(1.5)
              for tt in range(4):
                  ts_ = slice(512 * g + 128 * tt, 512 * g + 128 * tt + 128)
                  for c in range(8):
                      MM(pbf(5)[:, 0:384], actT[:, c, ts_], wgp[:, c, C_V:C_V + 384], c == 0, c == 7, rA + [r_wgp], [rpb[5]])
                  CP("act", vtok[:, tt, :], pbf(5)[:, 0:384], [rpb[5]], [r_vtok])
                  if partial:
                      continue
                  for c in range(8):
                      MM(pbf(2)[:, 0:384], actT[:, c, ts_], wgp[:, c, C_G:C_G + 384], c == 0, c == 7, rA + [r_wgp], [rpb[2]])
                  ACT(gsil[:, tt, :], pbf(2)[:, 0:384], AF.Silu, [rpb[2]], [r_gsil])
                  TT("pool", gsil[:, tt, :], gsil[:, tt, :], gngbc.rearrange("p a b -> p (a b)"), ALU.mult, [r_gsil, r_gng], [r_gsil])
              stage_gate(1.6)
              for j in range(2):
                  if partial and g < NG - 1:
                      continue
                  for c in range(8):
                      MM(pbf(1 + j), wgp[:, c, C_PU + 128 * j:C_PU + 128 * j + 128], actT[:, c, gs], c == 0, c == 7,
                         rA + [r_wgp], [rpb[1 + j]])
                  CP("act", ubuf[:, j, 16:528], pbf(1 + j), [rpb[1 + j]], [r_ubuf])
              if not partial:
                  stage_gate(1.61)
                  TT("dve", s2[:, :, 1:528], ubuf[:, :, 1:528], ubuf[:, :, 0:527], ALU.add, [r_ubuf], [r_s2])
                  TT("dve", s4[:, :, 3:528], s2[:, :, 3:528], s2[:, :, 1:526], ALU.add, [r_s2], [r_s4])
                  TT("dve", s8[:, 7:528], s4[:, 1, 7:528], s4[:, 1, 3:524], ALU.add, [r_s4], [r_s8])
                  TT("dve", s16[:, 15:528], s8[:, 15:528], s8[:, 7:520], ALU.add, [r_s8], [r_s16])
                  stage_gate(1.62)
                  for (plo, phi, j, src, rs) in ((0, 64, 0, s2[0:64, 0, 16:528], r_s2), (64, 128, 0, s4[64:128, 0, 16:528], r_s4),
                                                 (0, 64, 1, s8[0:64, 16:528], r_s8), (64, 128, 1, s16[64:128, 16:528], r_s16)):
                      STT(pooled[plo:phi, j, :], src, invw[plo:phi, j:j + 1], ubuf[plo:phi, j, 16:528], ALU.mult, ALU.subtract,
                          [rs, r_ubuf, r_const], [r_pooled])
                      if g == 0:
                          src16 = src[:, 0:16]
                          TT("dve", ptmp[plo:phi, j, :], src16, (invcnt0 if X0 else invcnt)[plo:phi, j, :], ALU.mult, [rs, r_const], [r_ptmp])
                          TT("dve", pooled[plo:phi, j, 0:16], ptmp[plo:phi, j, :], ubuf[plo:phi, j, 16:32], ALU.subtract,
                             [r_ptmp, r_ubuf], [r_pooled])
                  stage_gate(1.63)
                  for j in range(2):
                      MM(pbf(1 + j), poolbd[:, j, :], pooled[:, j, :], True, True, [r_poolbd, r_pooled], [rpb[1 + j]])
                      ACT(mixgp[:, 3 + j, :], pbf(1 + j), AF.Identity, [rpb[1 + j], r_pscol], [r_mixgp], scale=pscol[:, j:j + 1])
              stage_gate(1.64)
              if g == NG - 1:
                  if X0:
                      DMA("sp", oud[l], ubuf[:, :, 512:528], [r_ubuf], [r_ou])
              elif not partial:
                  for j in range(2):
                      CP("act", ubuf[:, j, 0:16], ubuf[:, j, 512:528], [r_ubuf], [r_ubuf])
              stage_gate(1.7)
              pendA = [None]; pendB = [None]
              for tt in range(4):
                  tsl = slice(128 * tt, 128 * tt + 128)
                  at_ps = pbf(0).rearrange("p (a b) -> p a b", a=4)
                  for hh in range(4):
                      if partial:
                          break
                      j, sl = hh // 2, 64 * (hh % 2)
                      MM(at_ps[:, hh, :], kin[j][0][sl:sl + 64, tsl], qde[j][0][sl:sl + 64, tsl], True, False,
                         [kin[j][1], qde[j][1]], [rpb[0]])
                      MM(at_ps[:, hh, :], kin[j][0][sl:sl + 64, tsl], qdo[j][0][sl:sl + 64, tsl], False, True,
                         [kin[j][1], qdo[j][1]], [rpb[0]])
                  if not partial:
                      TT("dve", attm.rearrange("p a b -> p (a b)"), pbf(0), glamask, ALU.mult, [rpb[0], r_c2], [r_attm])
                  if pendA[0] is not None:
                      pendA[0](); pendA[0] = None
                  o_ps = pbf(3)[:, 0:384].rearrange("p (a b) -> p a b", a=4)
                  o2_ps = pbf(5)[:, 0:384].rearrange("p (a b) -> p a b", a=4)
                  for par in range(2):
                      ch = 8 * g + 2 * tt + par
                      rows = slice(64 * par, 64 * par + 64)
                      Scur, r_Scur = Sbf[sb_i]
                      Snxt, r_Snxt = Sbf[1 - sb_i]
                      qsrc = qde if par == 0 else qdo
                      for hh in range(4):
                          if partial:
                              break
                          j, sl = hh // 2, 64 * (hh % 2)
                          if par == 0:
                              MM(o_ps[:, hh, :], attm[:, hh, :], vtok[:, tt, 96 * hh:96 * hh + 96], True, False,
                                 [r_attm, r_vtok], [rpb[3]])
                              MM(o_ps[:, hh, :], qsrc[j][0][sl:sl + 64, tsl], Scur[sl:sl + 64, j, :], False, True,
                                 [qsrc[j][1], r_Scur], [rpb[3]])
                          else:
                              MM(o2_ps[:, hh, :], qsrc[j][0][sl:sl + 64, tsl], Scur[sl:sl + 64, j, :], True, True,
                                 [qsrc[j][1], r_Scur], [rpb[5]])
                      kv_acc = pbf(4)[:, 0:192].rearrange("p (a b) -> p a b", a=2)
                      for hh in range(4):
                          j, sl = hh // 2, 64 * (hh % 2)
                          MM(kv_acc[sl:sl + 64, j, :], kintok[rows, tt, j, sl:sl + 64], vtok[rows, tt, 96 * hh:96 * hh + 96],
                             True, True, [r_kintok, r_vtok], [rpb[4]])
                      TT("dve", stmp, S32, kv_acc, ALU.add, [r_S32, rpb[4]], [r_stmp])
                      col = 64 * (2 * tt + par) + 63
                      for j in range(2):
                          TS("dve", S32[:, j, :], stmp[:, j, :], eb[j][0][:, col:col + 1], None, ALU.mult, None,
                             [r_stmp, eb[j][1]], [r_S32])
                          if not partial:
                              ACT(Snxt[:, j, :], stmp[:, j, :], AF.Identity, [r_stmp, eb[j][1]], [r_Snxt], scale=eb[j][0][:, col:col + 1])
                      sb_i = 1 - sb_i
                  if partial:
                      continue
                  if pendB[0] is not None:
                      pendB[0](); pendB[0] = None

                  def postA(tt=tt, o_ps=o_ps, o2_ps=o2_ps):
                      CP("act", osb, o_ps, [rpb[3]], [r_osb])
                      TT("dve", osb, osb, o2_ps, ALU.add, [r_osb, rpb[5]], [r_osb])
                      TT("dve", osq, osb, osb, ALU.mult, [r_osb], [r_osq])
                      RED(oss, osq, ALU.add, [r_osq], [r_oss])
                      ACT(orstd, oss, AF.Sqrt, [r_oss], [r_orstd], scale=1.0 / 96, bias=EPS)
                      RECIP(orstd, orstd, [r_orstd], [r_orstd])
                      for hh in range(4):
                          STT(ogla[:, 96 * hh:96 * hh + 96], osb[:, hh, :], orstd[:, hh:hh + 1], gsil[:, tt, 96 * hh:96 * hh + 96],
                              ALU.mult, ALU.mult, [r_osb, r_orstd, r_gsil], [r_ogla])

                  def postB(tsl=tsl):
                      og_ps = pbb(7).rearrange("p (c k) -> p c k", c=8)
                      for c in range(3):
                          TR(og_ps[:, c, :], ogla[:, 128 * c:128 * c + 128], [r_ogla], [rpb[7]])
                      CP("act", mixgp[:, 0:3, tsl], og_ps[:, 0:3, :], [rpb[7]], [r_mixgp])

                  pendA[0] = postA
                  pendB[0] = postB
              if pendA[0] is not None:
                  pendA[0](); pendA[0] = None
              if pendB[0] is not None:
                  pendB[0](); pendB[0] = None
              stage_gate(1.8)
              for tt in range(4):
                  if partial:
                      break
                  t = 4 * g + tt
                  for half in range(2):
                      pi = 1 + half
                      for c in range(5):
                          MM(pbf(pi), mixgp[:, c, 128 * tt:128 * tt + 128], wout_gp[:, c, 512 * half:512 * half + 512],
                             c == 0, c == 4, [r_mixgp, r_wout_gp], [rpb[pi]])
                      add_to_h(t, half, pbf(pi), rpb[pi])
              stage_gate(1.9 + 0.01 * g)
          stage_gate(1.95)
          if X0:
              DMA("sp", osd[l], S32, [r_S32], [r_os])
          P.fence(); A.release(m)
          if stages < 3:
              raise _Stop()

          m = A.mark()
          wsb, r_wsb = A.alloc("wsb", [8, 1152], BF16)
          wout_sb, r_wout_sb = A.alloc("wout_sb", [3, D], BF16)
          kT, r_kTprev = A.alloc("kT", [3, 2 * T], BF16)
          r_kTown = P.regs("kTown", NG)
          vall, r_vprev = A.alloc("vall", [32, 384], BF16)
          r_vown = P.regs("vown", NG)
          qT, _ = A.alloc("qT", [3, T], BF16)
          r_qT = P.regs("qT", NG)
          DMA("pool", wsb, w_in[l][:, O_SQ:O_SQ + 1152].rearrange(KC, p=128), [], [r_wsb])
          DMA("pool", wout_sb, w_out[l, 384:768, :].rearrange(KC, p=128), [], [r_wout_sb])
          if not X0:
              DMA("sp", kT[:, :, 0:T], xk[l], [r_ok], [r_kTprev])
              DMA("sp", vall[:, 0:16, :], xv[l], [r_ov], [r_vprev])
          for g in range(NG):
              gs = slice(512 * g, 512 * g + 512)
              rA = [ractT[g]]
              for pr in range(3):
                  if not partial:
                      for c in range(8):
                          MM(pbf(0), wsb[:, c, 128 * pr:128 * pr + 128], actT[:, c, gs], c == 0, c == 7, rA + [r_wsb], [rpb[0]])
                      ACT(qT[:, pr, gs], pbf(0), AF.Copy, [rpb[0]], [r_qT[g]], scale=0.125)
                  for c in range(8):
                      MM(pbf(1), wsb[:, c, 384 + 128 * pr:384 + 128 * pr + 128], actT[:, c, gs], c == 0, c == 7, rA + [r_wsb], [rpb[1]])
                  CP("dve", kT[:, pr, T + 512 * g:T + 512 * g + 512], pbf(1), [rpb[1]], [r_kTown[g]])
              for tt in range(4):
                  t = 4 * g + tt
                  ts_ = slice(128 * t, 128 * t + 128)
                  for c in range(8):
                      MM(pbf(2)[:, 0:384], actT[:, c, ts_], wsb[:, c, 768:1152], c == 0, c == 7, rA + [r_wsb], [rpb[2]])
                  CP("act", vall[:, 16 + t, :], pbf(2)[:, 0:384], [rpb[2]], [r_vown[g]])
          if X0:
              DMA("sp", ok[l], kT[:, :, T:2 * T], r_kTown, [r_ok])
              DMA("sp", ov[l], vall[:, 16:32, :], r_vown, [r_ov])
          if partial:
              raise _SegEnd()
          e_t = [A.alloc(f"e{i}", [512], F32) for i in range(3)]
          sp_t = [A.alloc(f"sp{i}", [512], BF16) for i in range(2)]
          en_t = [A.alloc(f"en{i}", [512], BF16) for i in range(2)]
          a_t = [A.alloc(f"a{i}", [512], BF16) for i in range(2)]
          spsum_t = [A.alloc(f"spsum{i}", [512], BF16) for i in range(2)]
          osbT, r_osbT = A.alloc("osbT", [3, 512], BF16)
          it = 0
          NFILL = 2
          for g in range(NG):
              gs = slice(512 * g, 512 * g + 512)
              for pr in range(3):
                  o_ps, r_o = pbf(4), rpb[4]
                  for par in range(2):
                      rows = slice(64 * par, 64 * par + 64)
                      hd = 2 * pr + par
                      blocks = [(16 + 4 * g + j, j) for j in (3, 2, 1, 0)] + [(kb, None) for kb in range(16 + 4 * g - 1, (15 if X0 else -1), -1)]
                      MSET("dve", spsum_t[0][0], 0.0, [spsum_t[0][1]])
                      nb = len(blocks)

                      def bufs(bi):
                          i2 = bi % 2
                          return (pbf(i2), rpb[i2], pbf(2 + i2), rpb[2 + i2]) + e_t[bi % 3] + sp_t[i2] + en_t[i2] + a_t[i2]

                      def kv_regs(kb):
                          if kb >= 16:
                              return [r_kTown[(kb - 16) // 4]], [r_vown[(kb - 16) // 4]]
                          return [r_kTprev], [r_vprev]

                      def sb_z(bi):
                          kb, dj = blocks[bi]
                          z_ps, r_z, n_ps, r_n, e, r_e, sp, r_sp, en, r_en, a, r_a = bufs(bi)
                          rk, rv = kv_regs(kb)
                          MM(z_ps, kT[rows, pr, 128 * kb:128 * kb + 128], qT[rows, pr, gs], True, True, rk + [r_qT[g]], [r_z])
                          if kb >= 16:
                              ACT(e, z_ps, AF.Exp, [r_z], [r_e])
                          else:
                              ACT(e, z_ps, AF.Exp, [r_z, r_const], [r_e], bias=prevbias[:, 0:1])
                          if dj is not None:
                              w = 128 * (dj + 1)
                              TT("dve", e[:, 0:w], e[:, 0:w], sbmask[:, 512 - w:512], ALU.mult, [r_e, r_const], [r_e])
                          ACT(sp, e, AF.Ln, [r_e], [r_sp], bias=1.0)

                      def sb_n(bi):
                          z_ps, r_z, n_ps, r_n, e, r_e, sp, r_sp, en, r_en, a, r_a = bufs(bi)
                          cur, r_cur = spsum_t[bi % 2]
                          nxt, r_nxt = spsum_t[(bi + 1) % 2]
                          MM(n_ps, tri, sp, True, bi == 0, [r_const, r_sp], [r_n])
                          if bi > 0:
                              MM(n_ps, ones, cur, False, True, [r_const, r_cur], [r_n])
                          ACT(en, n_ps, AF.Exp, [r_n], [r_en], scale=-1.0)
                          if bi + 1 < nb:
                              TT("dve", nxt, cur, sp, ALU.add, [r_cur, r_sp], [r_nxt])
                          TT("dve", a, e, en, ALU.mult, [r_e, r_en], [r_a])

                      def sb_fill(n):
                          for _ in range(n):
                              MM(pbf(5), ones, sbmask, True, True, [r_const], [rpb[5]])

                      def sb_av(bi):
                          kb, dj = blocks[bi]
                          z_ps, r_z, n_ps, r_n, e, r_e, sp, r_sp, en, r_en, a, r_a = bufs(bi)
                          rk, rv = kv_regs(kb)
                          MM(o_ps[rows, :], vall[:, kb, 64 * hd:64 * hd + 64], a, bi == 0, bi == nb - 1, rv + [r_a], [r_o])

                      sb_z(0)
                      for bi in range(nb):
                          if bi + 1 < nb:
                              sb_z(bi + 1)
                          sb_n(bi)
                          if bi >= 1:
                              sb_fill(NFILL)
                              sb_av(bi - 1)
                      sb_av(nb - 1)
                  CP("act", osbT[:, pr, :], o_ps, [r_o], [r_osbT])
              for tt in range(4):
                  t = 4 * g + tt
                  for half in range(2):
                      pi = 2 + half
                      for c in range(3):
                          MM(pbf(pi), osbT[:, c, 128 * tt:128 * tt + 128], wout_sb[:, c, 512 * half:512 * half + 512],
                             c == 0, c == 2, [r_osbT, r_wout_sb], [rpb[pi]])
                      add_to_h(t, half, pbf(pi), rpb[pi])
          P.fence(); A.release(m)
          if stages < 4:
              raise _Stop()

          m = A.mark()
          norm_to_T(ng[l, 1])
          P.fence(); A.release(m)
          m = A.mark()
          cw, r_cw = A.alloc("cw", [NT, 32], F32)
          wrt, r_wrt = A.alloc("wrt", [8, 36], BF16)
          brbc, r_brbc = A.alloc("brbc", [36], F32)
          DMA("pool", wrt, wr[l].rearrange(KC, p=128), [], [r_wrt])
          DMA("sp", brbc, br[l].partition_broadcast(128), [], [r_brbc])
          NRB = 3
          rt = {}
          for nm, shp in (("lg", [36]), ("gmax", [1]), ("ngmax", [1]), ("gex", [4]), ("gsum", [1]), ("gw", [1]), ("oh", [4]),
                          ("esel", [8]), ("m1", [1]), ("mk1", [8]), ("e2", [8]), ("m2", [1]), ("mk2", [8]), ("dd", [1]),
                          ("w1", [1]), ("w2", [1]), ("cs", [8])):
              rt[nm] = [A.alloc(f"rt_{nm}{i}", shp, F32) for i in range(NRB)]

          def router_a(t):
              ts_ = slice(128 * t, 128 * t + 128)
              pi = t % 2
              for c in range(8):
                  MM(pbf(pi)[:, 0:36], actT[:, c, ts_], wrt[:, c, :], c == 0, c == 7, [ractT[t // 4], r_wrt], [rpb[pi]])
              lg, r_lg = rt["lg"][t % NRB]
              gmax, r_gmax = rt["gmax"][t % NRB]; ngmax, r_ngmax = rt["ngmax"][t % NRB]
              gex, r_gex = rt["gex"][t % NRB]; gsum, r_gsum = rt["gsum"][t % NRB]
              TT("dve", lg, pbf(pi)[:, 0:36], brbc, ALU.add, [rpb[pi], r_brbc], [r_lg])
              RED(gmax, lg[:, 0:4], ALU.max, [r_lg], [r_gmax])
              TS("dve", ngmax, gmax, -1.0, None, ALU.mult, None, [r_gmax], [r_ngmax])
              ACT(gex, lg[:, 0:4], AF.Exp, [r_lg, r_ngmax], [r_gex, r_gsum], bias=ngmax[:, 0:1], accum_out=gsum)

          def router_b(t):
              b_ = t % NRB
              lg, r_lg = rt["lg"][b_]; gmax, r_gmax = rt["gmax"][b_]; gsum, r_gsum = rt["gsum"][b_]
              gw, r_gw = rt["gw"][b_]; oh, r_oh = rt["oh"][b_]; esel, r_esel = rt["esel"][b_]; m1, r_m1 = rt["m1"][b_]
              mk1, r_mk1 = rt["mk1"][b_]; e2, r_e2 = rt["e2"][b_]; m2, r_m2 = rt["m2"][b_]; mk2, r_mk2 = rt["mk2"][b_]
              dd, r_dd = rt["dd"][b_]; w1, r_w1 = rt["w1"][b_]
              TS("dve", oh, lg[:, 0:4], gmax[:, 0:1], None, ALU.is_equal, None, [r_lg, r_gmax], [r_oh])
              TS("dve", esel, lg[:, 4:12], oh[:, 0:1], None, ALU.mult, None, [r_lg, r_oh], [r_esel])
              for gi in range(1, 4):
                  STT(esel, lg[:, 4 + 8 * gi:12 + 8 * gi], oh[:, gi:gi + 1], esel, ALU.mult, ALU.add, [r_lg, r_oh, r_esel], [r_esel])
              RED(m1, esel, ALU.max, [r_esel], [r_m1])
              TS("dve", mk1, esel, m1[:, 0:1], None, ALU.is_equal, None, [r_esel, r_m1], [r_mk1])
              STT(e2, mk1, -1e30, esel, ALU.mult, ALU.add, [r_mk1, r_esel], [r_e2])
              RED(m2, e2, ALU.max, [r_e2], [r_m2])
              TS("dve", mk2, e2, m2[:, 0:1], None, ALU.is_equal, None, [r_e2, r_m2], [r_mk2])
              TT("dve", dd, m1, m2, ALU.subtract, [r_m1, r_m2], [r_dd])
              ACT(w1, dd, AF.Sigmoid, [r_dd], [r_w1])
              RECIP(gw, gsum, [r_gsum], [r_gw])

          def router_c(t):
              b_ = t % NRB
              gw, r_gw = rt["gw"][b_]; oh, r_oh = rt["oh"][b_]; mk1, r_mk1 = rt["mk1"][b_]; mk2, r_mk2 = rt["mk2"][b_]
              w1, r_w1 = rt["w1"][b_]; w2, r_w2 = rt["w2"][b_]; cs, r_cs = rt["cs"][b_]
              TT("dve", w1, w1, gw, ALU.mult, [r_w1, r_gw], [r_w1])
              TT("dve", w2, gw, w1, ALU.subtract, [r_gw, r_w1], [r_w2])
              TS("dve", cs, mk1, w1[:, 0:1], None, ALU.mult, None, [r_mk1, r_w1], [r_cs])
              STT(cs, mk2, w2[:, 0:1], cs, ALU.mult, ALU.add, [r_mk2, r_w2, r_cs], [r_cs])
              for gi in range(4):
                  TS("dve", cw[:, t, 8 * gi:8 * gi + 8], cs, oh[:, gi:gi + 1], None, ALU.mult, None, [r_cs, r_oh], [r_cw])

          for t in range(NT + 2):
              if t < NT:
                  router_a(t)
              if 1 <= t <= NT:
                  router_b(t - 1)
              if t >= 2:
                  router_c(t - 2)
          if stages < 5:
              raise _Stop()

          NBUF = 2
          wgu_b = [A.alloc(f"wgu{i}", [2, 8, 512], BF16) for i in range(NBUF)]
          wd_b = [A.alloc(f"wd{i}", [2, 2, D], BF16) for i in range(NBUF)]
          sg_t = [A.alloc(f"sg{i}", [256], F32) for i in range(2)]
          hid_t = [A.alloc(f"hid{i}", [256], BF16) for i in range(2)]
          hidT_t = [A.alloc(f"hidT{i}", [2, 128], BF16) for i in range(2)]
          steps = [(ep, t, s_) for ep in range(NEXP // 2) for t in range(NT) for s_ in range(2)]

          def moe_ab(k):
              ep, t, s_ = steps[k]
              wgu, r_wgu = wgu_b[ep % NBUF]
              wdd, r_wd = wd_b[ep % NBUF]
              if t == 0 and s_ == 0:
                  for s2_ in range(2):
                      e_ = 2 * ep + s2_
                      DMA("pool", wgu[:, s2_, :, 0:256], wg[l, e_].rearrange(KC, p=128), [], [r_wgu])
                      DMA("pool", wgu[:, s2_, :, 256:512], wu[l, e_].rearrange(KC, p=128), [], [r_wgu])
                      DMA("pool", wdd[:, s2_, :, :], wd[l, e_].rearrange(KC, p=128), [], [r_wd])
              e_ = 2 * ep + s_
              i2 = k % 2
              ts_ = slice(128 * t, 128 * t + 128)
              ab, r_ab = pbf(i2), rpb[i2]
              for c in range(8):
                  MM(ab, actT[:, c, ts_], wgu[:, s_, c, :], c == 0, c == 7, [ractT[t // 4], r_wgu], [r_ab])
              sg, r_sg = sg_t[i2]; hid, r_hid = hid_t[i2]
              ACT(sg, ab[:, 0:256], AF.Silu, [r_ab], [r_sg])
              STT(hid, ab[:, 256:512], cw[:, t, e_:e_ + 1], sg, ALU.mult, ALU.mult, [r_ab, r_cw, r_sg], [r_hid])

          def moe_tr(k):
              i2 = k % 2
              hid, r_hid = hid_t[i2]; hidT, r_hidT = hidT_t[i2]
              tp = pbb(6 + i2).rearrange("p (c k) -> p c k", c=8)
              for f in range(2):
                  TR(tp[:, f, :], hid[:, 128 * f:128 * f + 128], [r_hid], [rpb[6 + i2]])
              CP("act", hidT, tp[:, 0:2, :], [rpb[6 + i2]], [r_hidT])

          def moe_down(k):
              ep, t, s_ = steps[k]
              wdd, r_wd = wd_b[ep % NBUF]
              hidT, r_hidT = hidT_t[k % 2]
              yb = 2 + 2 * (t % 2)
              for half in range(2):
                  for f in range(2):
                      MM(pbf(yb + half), hidT[:, f, :], wdd[:, s_, f, 512 * half:512 * half + 512],
                         s_ == 0 and f == 0, s_ == 1 and f == 1, [r_hidT, r_wd], [rpb[yb + half]])
              if s_ == 1:
                  for half in range(2):
                      add_to_h(t, half, pbf(yb + half), rpb[yb + half])

          nst = len(steps)
          moe_ab(0)
          for k in range(nst):
              if k + 1 < nst:
                  moe_ab(k + 1)
              moe_tr(k)
              if k >= 1:
                  moe_down(k - 1)
          moe_down(nst - 1)
          P.fence(); A.release(m)
          if stages < 6:
              raise _Stop()

          m = A.mark()
          norm_to_T(ng[l, 2])
          P.fence(); A.release(m)
          m = A.mark()
          gwt, r_gwt = A.alloc("gwt", [8, D], BF16)
          pwt, r_pwt = A.alloc("pwt", [2, D], BF16)
          gbbc, r_gbbc = A.alloc("gbbc", [D], F32)
          DMA("pool", gwt, pgw[l].rearrange(KC, p=128), [], [r_gwt])
          DMA("pool", pwt, ppw[l].rearrange(KC, p=128), [], [r_pwt])
          DMA("sp", gbbc, pgb[l].partition_broadcast(128), [], [r_gbbc])
          pt_t = [A.alloc(f"pt{i}", [256], BF16) for i in range(2)]
          pT_t = [A.alloc(f"pT{i}", [2, 128], BF16) for i in range(2)]
          gate_t = [A.alloc(f"gate{i}", [D], F32) for i in range(2)]
          for t in range(NT):
              ts_ = slice(128 * t, 128 * t + 128)
              i2 = t % 2
              pt, r_pt = pt_t[i2]; pT, r_pT = pT_t[i2]; gate, r_gate = gate_t[i2]
              DMA("pool", pt, p_in[l, 128 * t:128 * t + 128, :], [], [r_pt])
              tp = pbb(6 + i2).rearrange("p (c k) -> p c k", c=8)
              for f in range(2):
                  TR(tp[:, f, :], pt[:, 128 * f:128 * f + 128], [r_pt], [rpb[6 + i2]])
              CP("act", pT, tp[:, 0:2, :], [rpb[6 + i2]], [r_pT])
              def ple_gate_mm(half):
                  hs = slice(512 * half, 512 * half + 512)
                  gp, r_gp = pbf(half), rpb[half]
                  for c in range(8):
                      MM(gp, actT[:, c, ts_], gwt[:, c, hs], c == 0, c == 7, [ractT[t // 4], r_gwt], [r_gp])

              def ple_e_mm(half):
                  hs = slice(512 * half, 512 * half + 512)
                  ep_, r_ep = pbf(2 + 2 * i2 + half), rpb[2 + 2 * i2 + half]
                  for f in range(2):
                      MM(ep_, pT[:, f, :], pwt[:, f, hs], f == 0, f == 1, [r_pT, r_pwt], [r_ep])

              ple_gate_mm(0); ple_e_mm(0); ple_e_mm(1); ple_gate_mm(1)
              for half in range(2):
                  hs = slice(512 * half, 512 * half + 512)
                  gp, r_gp = pbf(half), rpb[half]
                  ep_, r_ep = pbf(2 + 2 * i2 + half), rpb[2 + 2 * i2 + half]
                  r_gh = P.reg(f"gate{i2}h{half}")
                  TT("dve", gate[:, hs], gp, gbbc[:, hs], ALU.add, [r_gp, r_gbbc], [r_gh])
                  ACT(gate[:, hs], gate[:, hs], AF.Sigmoid, [r_gh], [r_gh])
                  TT("dve", gate[:, hs], gate[:, hs], ep_, ALU.mult, [r_gh, r_ep], [r_gh])
                  TT("pool", h[:, t, hs], h[:, t, hs], gate[:, hs], ALU.add, [rh[t], r_gh], [rh[t]])
          P.fence(); A.release(m)
      except _SegEnd:
        P.fence(); A.release(m_layer)
      except _Stop:
        P.fence(); A.release(m_layer)
        stopped = True

    m = A.mark()
    if stages <= 0:
        P.emit(outputs_final=[])
        es.close()
        return nc
    gbc, r_g = A.alloc("fgbc", [D], F32)
    junk, r_junk = A.alloc("fjunk", [D], BF16)
    ssq, r_ss = A.alloc("fssq", [NT], F32)
    rstd, r_rstd = A.alloc("frstd", [NT], F32)
    yt = [A.alloc(f"yt{i}", [D], F32) for i in range(2)]
    DMA("sp", gbc, fg.partition_broadcast(128), [], [r_g])
    for t in range(NT):
        ACT(junk, h[:, t, :], AF.Square, [rh[t]], [r_junk, r_ss], accum_out=ssq[:, t:t + 1])
    ACT(rstd, ssq, AF.Sqrt, [r_ss], [r_rstd], scale=1.0 / D, bias=EPS)
    RECIP(rstd, rstd, [r_rstd], [r_rstd])
    for t in range(NT):
        yb_, r_yb = yt[t % 2]
        STT(yb_, h[:, t, :], rstd[:, t:t + 1], gbc, ALU.mult, ALU.mult, [rh[t], r_rstd, r_g], [r_yb])
        DMA("sp", y_out[128 * t:128 * t + 128, :], yb_, [r_yb], [r_yout])
    P.emit(outputs_final=[r_yout])
    print("arena high-water KiB:", A.hi / 1024, "ops:", P.n, flush=True)
    es.close()
    return nc


def _consts():
    c = {}
    c["c_ident"] = np.eye(128, dtype=np.float32).astype(NPBF)
    jj = np.arange(128)
    c["c_tri"] = (jj[:, None] >= jj[None, :]).astype(np.float32).astype(NPBF)
    c["c_ones"] = np.ones((128, 128), np.float32).astype(NPBF)
    sb = np.zeros((128, 512), np.float32)
    sb[:, 384:512] = (jj[:, None] < jj[None, :])
    c["c_sbmask"] = sb.astype(NPBF)
    blk = ((jj[:, None] // 64) == (jj[None, :] // 64)) & (jj[:, None] <= jj[None, :])
    c["c_glamask"] = np.tile(blk.astype(np.float32), (1, 4)).astype(NPBF)
    col = np.arange(512)
    c["c_scanmask"] = np.tile((col % 64 != 0).astype(np.float32)[None, :], (128, 1)).astype(NPBF)
    ev = ((col // 64) % 2 == 0).astype(np.float32)
    c["c_evenmask"] = np.tile(ev[None, :], (128, 1)).astype(NPBF)
    c["c_oddmask"] = np.tile((1 - ev)[None, :], (128, 1)).astype(NPBF)
    invw = np.zeros((128, 2), np.float32)
    invw[0:64, 0] = 1 / 2; invw[64:, 0] = 1 / 4; invw[0:64, 1] = 1 / 8; invw[64:, 1] = 1 / 16
    c["c_invw"] = invw
    c["c_invcnt0"] = _role(0)["r_invcnt"]
    return c


def _role(r):
    d = {}
    d["r_prevbias"] = np.full((128, 1), 0.0 if r == 1 else -30000.0, np.float32)
    d["r_flag"] = np.full((128, 1), float(r), np.float32)
    w = np.zeros((128, 2), np.float32)
    w[0:64, 0] = 2; w[64:, 0] = 4; w[0:64, 1] = 8; w[64:, 1] = 16
    t = np.arange(16, dtype=np.float32)
    if r == 0:
        cnt = np.minimum(t[None, None, :] + 1.0, w[:, :, None])
    else:
        cnt = np.broadcast_to(w[:, :, None], (128, 2, 16))
    d["r_invcnt"] = (1.0 / cnt).astype(np.float32)
    return d


def _layer_weights(inp, ls):
    f = lambda k: np.ascontiguousarray(inp[k][ls])
    d = {}
    d["ng"] = np.ascontiguousarray(np.stack([inp["norm1_g"][ls], inp["norm2_g"][ls], inp["ple_norm_g"][ls]], axis=1))
    d["fg"] = np.ascontiguousarray(inp["final_norm_g"])
    d["w_in"] = f("w_in"); d["w_lr2"] = f("gla_w_lr2"); d["b_lr"] = f("gla_b_lr"); d["gng"] = f("gla_norm_g")
    d["pool_w"] = f("pool_w"); d["pool_s"] = f("pool_scale"); d["w_out"] = f("w_out")
    d["wr"] = np.ascontiguousarray(np.concatenate([inp["router_group_w"][ls], inp["router_exp_w"][ls]], axis=-1))
    d["br"] = np.ascontiguousarray(np.concatenate([inp["router_group_b"][ls], inp["router_exp_b"][ls]], axis=-1))
    d["wg"] = f("exp_w_gate"); d["wu"] = f("exp_w_up"); d["wd"] = f("exp_w_down")
    d["pgw"] = f("ple_gate_w"); d["pgb"] = f("ple_gate_b"); d["ppw"] = f("ple_proj_w")
    return d


_NC_CACHE = {}


def _get_nc(L, stages=99):
    key = (L, stages)
    if key not in _NC_CACHE:
        _NC_CACHE[key] = build(L, stages)
    return _NC_CACHE[key]


def make_maps(inp, L=DEPTH, batches=(0, 1, 2, 3), roles=(0, 1)):
    consts = _consts()
    lw = _layer_weights(inp, slice(0, L))
    maps = []
    for b in batches:
        for r in roles:
            m = dict(consts); m.update(lw); m.update(_role(r))
            m["h0a"] = np.ascontiguousarray(inp["x"][b, 0:T])
            m["pa"] = np.ascontiguousarray(inp["p"][0:L, b, 0:T, :])
            m["h0b"] = np.ascontiguousarray(inp["x"][b, r * T:(r + 1) * T])
            m["pb"] = np.ascontiguousarray(inp["p"][0:L, b, r * T:(r + 1) * T, :])
            maps.append(m)
    return maps


def kernel(**inputs):
    inp = {k: np.asarray(v) for k, v in inputs.items()}
    B = inp["x"].shape[0]
    nc = _get_nc(DEPTH)
    maps = make_maps(inp, DEPTH, tuple(range(B)), (0, 1))
    res = run_bass_kernel_spmd(nc, maps, core_ids=list(range(len(maps))))
    out = np.zeros((B, 2 * T, D), np.float32)
    i = 0
    for b in range(B):
        for r in (0, 1):
            out[b, r * T:(r + 1) * T] = res.results[i]["y_out"]
            i += 1
    return out
```

```python
import numpy as np
import ml_dtypes
from contextlib import ExitStack
import concourse.bass as bass
import concourse.mybir as mybir
from concourse.bass_utils import run_bass_kernel_spmd
from concourse.alu_op_type import AluOpType as ALU

AF = mybir.ActivationFunctionType
AX = mybir.AxisListType
F32 = mybir.dt.float32
BF16 = mybir.dt.bfloat16
NPBF = ml_dtypes.bfloat16

D = 1024
T = 2048
NT = 16
NG = 4
DEPTH = 2
EPS = 1e-6
NEXP = 32
O_GQ, O_GK, O_GV, O_GO, O_LR, O_SQ, O_SK, O_SV, O_PU = 0, 192, 384, 768, 1152, 1168, 1552, 1936, 2320


class Reg:
    __slots__ = ("name", "writer", "readers", "dsem", "dcount", "lastdma", "maxw", "rt")

    def __init__(self, name):
        self.name = name
        self.writer = None
        self.readers = []
        self.dsem = None
        self.dcount = 0
        self.lastdma = None
        self.maxw = 0
        self.rt = None


class Op:
    __slots__ = ("eng", "fn", "deps", "signal", "is_dma", "dreg", "dval", "rank", "idx")


class Prog:
    ENG = ("pe", "act", "dve", "pool", "sp")

    def __init__(self, nc):
        self.nc = nc
        self.ops = {e: [] for e in self.ENG}
        self.n = 0
        self.dma_regs = []
        self.all_regs = []
        self.fence_deps = []
        self.fence_pending = set()
        self.regcache = {}

    def reg(self, name):
        if name in self.regcache:
            return self.regcache[name]
        r = Reg(name)
        self.regcache[name] = r
        self.all_regs.append(r)
        return r

    def regs(self, name, n):
        return [self.reg(f"{name}{i}") for i in range(n)]

    def fence(self):
        deps = []
        for e in self.ENG:
            for o in reversed(self.ops[e]):
                if not o.is_dma:
                    deps.append(o)
                    break
        for r in self.dma_regs:
            if r.lastdma is not None:
                deps.append(r.lastdma)
        for d in deps:
            d.signal = True
        self.fence_deps = deps
        self.fence_pending = set(self.ENG)
        for r in self.all_regs:
            r.writer = None
            r.readers = []

    def op(self, eng, fn, reads=(), writes=(), dma=False, accum=False):
        o = Op()
        o.eng = eng
        o.fn = fn
        o.is_dma = dma
        o.signal = False
        o.dreg = None
        o.dval = 0
        o.rank = 0
        o.idx = self.n
        self.n += 1
        deps = []
        if eng in self.fence_pending:
            self.fence_pending.discard(eng)
            deps.extend(self.fence_deps)
        for r in reads:
            if r.writer is not None:
                deps.append(r.writer)
        for r in writes:
            w = r.writer
            if w is not None:
                if accum and w.eng == eng and not w.is_dma:
                    pass
                elif dma and w.is_dma:
                    pass
                else:
                    deps.append(w)
            deps.extend(r.readers)
        seen = set()
        dl = []
        for d in deps:
            if d.idx in seen:
                continue
            seen.add(d.idx)
            if d.is_dma:
                val = 16 * d.dreg.dcount
                d.dreg.maxw = max(d.dreg.maxw, val)
                dl.append((d, val))
            else:
                dl.append((d, None))
                d.signal = True
        o.deps = dl
        if dma:
            r = writes[0]
            if r.dsem is None:
                self.dma_regs.append(r)
                r.dsem = True
            if r.maxw > 0:
                dl.append((r.lastdma, r.maxw))
            r.dcount += 1
            r.lastdma = o
            o.dreg = r
            o.dval = 16 * r.dcount
        for r in reads:
            r.readers.append(o)
        for r in writes:
            r.writer = o
            r.readers = []
        self.ops[eng].append(o)
        return o

    def emit(self, outputs_final=()):
        nc = self.nc
        with ExitStack() as es:
            esem = {e: es.enter_context(nc.semaphore(f"s_{e}")) for e in self.ENG}
            for i, r in enumerate(self.dma_regs):
                r.dsem = es.enter_context(nc.semaphore(f"d{i}_{r.name}"))
            for e in self.ENG:
                k = 0
                for o in self.ops[e]:
                    if o.is_dma:
                        continue
                    if o.signal:
                        k += 1
                        o.rank = k
            block = es.enter_context(nc.Block())

            def run(ename, eng):
                waited = {}
                for o in self.ops[ename]:
                    for d, dv in o.deps:
                        if d.is_dma:
                            sem, val = d.dreg.dsem, dv
                        else:
                            sem, val = esem[d.eng], d.rank
                        key = id(sem)
                        if waited.get(key, 0) >= val:
                            continue
                        waited[key] = val
                        eng.wait_ge(sem, val)
                    ins = o.fn(eng)
                    if o.is_dma:
                        ins.then_inc(o.dreg.dsem, 16)
                    elif o.signal:
                        ins.then_inc(esem[ename], 1)
                if ename == "sp":
                    for r in outputs_final:
                        if r.dcount:
                            eng.wait_ge(r.dsem, 16 * r.dcount)

            block.tensor(lambda eng: run("pe", eng))
            block.scalar(lambda eng: run("act", eng))
            block.vector(lambda eng: run("dve", eng))
            block.gpsimd(lambda eng: run("pool", eng))
            block.sync(lambda eng: run("sp", eng))


ARENA_BYTES = 202 * 1024


class Arena:
    def __init__(self, ap_bf16, P):
        self.ap = ap_bf16
        self.off = 0
        self.P = P
        self.hi = 0

    def alloc(self, name, free_shape, dt, reg=True):
        esz = 4 if dt == F32 else 2
        n = int(np.prod(free_shape))
        nbytes = (n * esz + 31) // 32 * 32
        assert self.off + nbytes <= ARENA_BYTES, (name, self.off, nbytes)
        v = self.ap[:, self.off // 2:(self.off + n * esz) // 2]
        if dt == F32:
            v = v.bitcast(F32)
        if len(free_shape) == 2:
            v = v.rearrange("p (a b) -> p a b", a=free_shape[0])
        elif len(free_shape) == 3:
            v = v.rearrange("p (a b c) -> p a b c", a=free_shape[0], b=free_shape[1])
        self.off += nbytes
        self.hi = max(self.hi, self.off)
        return (v, self.P.reg(name)) if reg else v

    def mark(self):
        return self.off

    def release(self, m):
        self.off = m


def build(L, stages=99):
    nc = bass.Bass("TRN2", target_bir_lowering=False)
    P = Prog(nc)

    def din(name, shape, dt=F32):
        return nc.dram_tensor(name, list(shape), dt, kind="ExternalInput").ap()

    def dout(name, shape, dt=F32):
        return nc.dram_tensor(name, list(shape), dt, kind="ExternalOutput").ap()

    h0 = din("h0a", [T, D]); h0b = din("h0b", [T, D])
    p_a = din("pa", [L, T, 256]); p_b = din("pb", [L, T, 256])
    ng = din("ng", [L, 3, D]); fg = din("fg", [D])
    w_in = din("w_in", [L, D, 2576]); w_lr2 = din("w_lr2", [L, 16, 192]); b_lr = din("b_lr", [L, 192])
    gng = din("gng", [L, 96]); pool_w = din("pool_w", [L, 4, 64, 64]); pool_s = din("pool_s", [L, 256])
    w_out = din("w_out", [L, D, D]); wr = din("wr", [L, D, 36]); br = din("br", [L, 36])
    wg = din("wg", [L, NEXP, D, 256]); wu = din("wu", [L, NEXP, D, 256]); wd = din("wd", [L, NEXP, 256, D])
    pgw = din("pgw", [L, D, D]); pgb = din("pgb", [L, D]); ppw = din("ppw", [L, 256, D])
    c_ident = din("c_ident", [128, 128], BF16); c_tri = din("c_tri", [128, 128], BF16)
    c_ones = din("c_ones", [128, 128], BF16)
    c_sbmask = din("c_sbmask", [128, 512], BF16); c_glamask = din("c_glamask", [128, 512], BF16)
    c_scanmask = din("c_scanmask", [128, 512], BF16); c_evenmask = din("c_evenmask", [128, 512], BF16)
    c_oddmask = din("c_oddmask", [128, 512], BF16)
    c_invw = din("c_invw", [128, 2])
    r_prevbias = din("r_prevbias", [128, 1]); r_flag = din("r_flag", [128, 1]); r_invcnt = din("r_invcnt", [128, 2, 16])
    c_invcnt0 = din("c_invcnt0", [128, 2, 16])

    def dscr(name, shape, dt=F32):
        return nc.dram_tensor(name, list(shape), dt).ap()

    xk = dscr("ex_k", [L, 128, 3, T], BF16); xv = dscr("ex_v", [L, 128, 16, 384], BF16)
    xs = dscr("ex_s", [L, 128, 2, 96]); xu = dscr("ex_u", [L, 128, 2, 16])
    ok, ov, osd, oud = xk, xv, xs, xu
    hsA = dscr("hsA", [T, D]); hsB = dscr("hsB", [T, D])
    y_out = dout("y_out", [T, D])
    r_hsA = P.reg("hsA"); r_hsB = P.reg("hsB")
    r_yout = P.reg("y_out"); r_ok = P.reg("ok"); r_ov = P.reg("ov")
    r_os = P.reg("os"); r_ou = P.reg("ou")

    es = ExitStack()
    arena_t = es.enter_context(nc.sbuf_tensor("arena", [128, ARENA_BYTES // 2], BF16))
    A = Arena(arena_t[:], P)
    pbank = [es.enter_context(nc.psum_tensor(f"pb{i}", [128, 512], F32)) for i in range(6)]
    pbank += [es.enter_context(nc.psum_tensor(f"pb{i}", [128, 1024], BF16)) for i in (6, 7)]
    rpb = P.regs("pb", 8)

    def pbf(i):
        assert i < 6
        return pbank[i][:]

    def pbb(i):
        assert i >= 6
        return pbank[i][:]

    def MM(out, lhsT, rhs, start, stop, rd, wr):
        rt = (lhsT.base_partition(), lhsT.partition_size())
        acc = all(r.rt is None or r.rt == rt for r in wr)
        P.op("pe", lambda e: e.matmul(out, lhsT=lhsT, rhs=rhs, start=start, stop=stop), rd, wr, accum=acc)
        for r in wr:
            r.rt = rt

    def TR(out, in_, rd, wr):
        rt = (0, 128)
        acc = all(r.rt is None or r.rt == rt for r in wr)
        P.op("pe", lambda e: e.transpose(out=out, in_=in_, identity=ident), rd + [r_const], wr, accum=acc)
        for r in wr:
            r.rt = rt

    def ACT(out, in_, func, rd, wr, **kw):
        P.op("act", lambda e: e.activation(out=out, in_=in_, func=func, **kw), rd, wr)

    def TT(eng, out, in0, in1, op, rd, wr):
        P.op(eng, lambda e: e.tensor_tensor(out=out, in0=in0, in1=in1, op=op), rd, wr)

    def TS(eng, out, in0, s1, s2, op0, op1, rd, wr):
        if op1 is None:
            P.op(eng, lambda e: e.tensor_scalar(out=out, in0=in0, scalar1=s1, scalar2=None, op0=op0), rd, wr)
        else:
            P.op(eng, lambda e: e.tensor_scalar(out=out, in0=in0, scalar1=s1, scalar2=s2, op0=op0, op1=op1), rd, wr)

    def STT(out, in0, scalar, in1, op0, op1, rd, wr):
        P.op("dve", lambda e: e.scalar_tensor_tensor(out=out, in0=in0, scalar=scalar, in1=in1, op0=op0, op1=op1), rd, wr)

    def CP(eng, out, in_, rd, wr):
        if eng == "act":
            ACT(out, in_, AF.Copy, rd, wr)
        else:
            P.op(eng, lambda e: e.tensor_copy(out=out, in_=in_), rd, wr)

    def MSET(eng, ap, val, wr):
        P.op(eng, lambda e: e.memset(ap, val), [], wr)

    def DMA(eng, out, in_, rd, wr):
        P.op(eng, lambda e: e.dma_start(out=out, in_=in_), rd, wr, dma=True)

    def RED(out, in_, op, rd, wr):
        P.op("dve", lambda e: e.tensor_reduce(out=out, in_=in_, axis=AX.X, op=op), rd, wr)

    def RECIP(out, in_, rd, wr):
        P.op("dve", lambda e: e.reciprocal(out=out, in_=in_), rd, wr)

    KC = "(c p) n -> p c n"

    h, _ = A.alloc("h", [NT, D], F32)
    rh = P.regs("h", NT)
    r_hload = P.reg("hload")
    r_const = P.reg("const")
    ident = A.alloc("ident", [128], BF16, reg=False)
    tri = A.alloc("tri", [128], BF16, reg=False)
    ones = A.alloc("ones", [128], BF16, reg=False)
    sbmask = A.alloc("sbmask", [512], BF16, reg=False)
    invw = A.alloc("invw", [2], F32, reg=False)
    prevbias = A.alloc("prevbias", [1], F32, reg=False)
    flag = A.alloc("flag", [1], F32, reg=False)
    invcnt = A.alloc("invcnt", [2, 16], F32, reg=False)
    invcnt0 = A.alloc("invcnt0", [2, 16], F32, reg=False)
    for dst, src in ((ident, c_ident), (tri, c_tri), (ones, c_ones), (sbmask, c_sbmask), (invw, c_invw),
                     (prevbias, r_prevbias), (flag, r_flag), (invcnt, r_invcnt),
                     (invcnt0, c_invcnt0)):
        DMA("sp", dst, src, [], [r_const])
    for q in range(4):
        DMA("sp", h[:, 4 * q:4 * q + 4, :], h0[512 * q:512 * q + 512, :].rearrange("(t p) d -> p t d", p=128),
            [], [r_hload] + rh[4 * q:4 * q + 4])

    actT, _ = A.alloc("actT", [8, T], BF16)
    ractT = P.regs("actT", NG)

    def norm_to_T(gsrc_ap):
        gbc, r_g = A.alloc("gbc", [D], F32)
        junk, r_junk = A.alloc("junk", [D], BF16)
        ssq, r_ss = A.alloc("ssq", [NT], F32)
        rstd, r_rstd = A.alloc("rstd", [NT], F32)
        xn = [A.alloc(f"xn{i}", [D], BF16) for i in range(2)]
        DMA("sp", gbc, gsrc_ap.partition_broadcast(128), [], [r_g])
        for t in range(NT):
            ACT(junk, h[:, t, :], AF.Square, [rh[t]], [r_junk, r_ss], accum_out=ssq[:, t:t + 1])
        ACT(rstd, ssq, AF.Sqrt, [r_ss], [r_rstd], scale=1.0 / D, bias=EPS)
        RECIP(rstd, rstd, [r_rstd], [r_rstd])
        for t in range(NT):
            xb, r_xb = xn[t % 2]
            STT(xb, h[:, t, :], rstd[:, t:t + 1], gbc, ALU.mult, ALU.mult, [rh[t], r_rstd, r_g], [r_xb])
            pi = 6 + t % 2
            pv = pbb(pi).rearrange("p (c k) -> p c k", c=8)
            for c in range(8):
                TR(pv[:, c, :], xb[:, 128 * c:128 * c + 128], [r_xb], [rpb[pi]])
            CP("act" if t % 2 == 0 else "dve", actT[:, :, 128 * t:128 * t + 128], pv, [rpb[pi]], [ractT[t // 4]])

    def add_to_h(t, half, ps_ap, rps):
        hs = h[:, t, 512 * half:512 * half + 512]
        TT("dve", hs, hs, ps_ap, ALU.add, [rh[t], rps], [rh[t]])

    class _Stop(Exception):
        pass

    class _SegEnd(Exception):
        pass

    def swap_h(store_ap, r_store, load_ap, r_load):
        if store_ap is not None:
            for q in range(4):
                DMA("sp", store_ap[512 * q:512 * q + 512, :].rearrange("(t p) d -> p t d", p=128), h[:, 4 * q:4 * q + 4, :],
                    rh[4 * q:4 * q + 4], [r_store])
        for q in range(4):
            DMA("sp", h[:, 4 * q:4 * q + 4, :], load_ap[512 * q:512 * q + 512, :].rearrange("(t p) d -> p t d", p=128),
                [r_load] if r_load is not None else [], [r_hload] + rh[4 * q:4 * q + 4])

    def stage_gate(x):
        if stages < x:
            raise _Stop()

    r_h0b = None
    segs = [(l, sg) for l in range(L) for sg in (0, 1)]
    stopped = False
    for (l, sg) in segs:
      if stopped:
          break
      X0 = (sg == 0)
      partial = X0 and (l == L - 1)
      p_in = p_a if X0 else p_b
      if (l, sg) == (0, 1):
          swap_h(hsA, r_hsA, h0b, None)
      elif sg == 0 and l > 0:
          swap_h(hsB, r_hsB, hsA, r_hsA)
      elif sg == 1 and l > 0:
          swap_h(None, None, hsB, r_hsB)
      P.fence()
      m_layer = A.mark()
      try:
          m = A.mark()
          if stages >= 1:
              norm_to_T(ng[l, 0])
          P.fence(); A.release(m)
          if stages <= 1:
              raise _Stop()
          m = A.mark()
          NC2 = 1552
          C_Q, C_K, C_V, C_G, C_LR, C_PU = 0, 256, 512, 896, 1280, 1296
          wgp, r_wgp = A.alloc("wgp", [8, NC2], BF16)
          wout_gp, r_wout_gp = A.alloc("wout_gp", [5, D], BF16)
          wlr2p, r_wl = A.alloc("wlr2p", [256], BF16)
          negb, r_negb = A.alloc("negb", [2], F32)
          gngbc, r_gng = A.alloc("gngbc", [4, 96], F32)
          poolbd, r_poolbd = A.alloc("poolbd", [2, 128], BF16)
          pscol, r_pscol = A.alloc("pscol", [2], F32)
          glamask = A.alloc("glamask", [512], BF16, reg=False)
          scanmask = A.alloc("scanmask", [512], BF16, reg=False)
          evenmask = A.alloc("evenmask", [512], BF16, reg=False)
          oddmask = A.alloc("oddmask", [512], BF16, reg=False)
          r_c2 = P.reg("c2")
          for dst, src in ((glamask, c_glamask), (scanmask, c_scanmask), (evenmask, c_evenmask), (oddmask, c_oddmask)):
              DMA("sp", dst, src, [], [r_c2])
          MSET("pool", wgp[:, :, 0:512], 0.0, [r_wgp])
          MSET("pool", wlr2p, 0.0, [r_wl])
          MSET("pool", negb, 0.0, [r_negb])
          MSET("pool", poolbd, 0.0, [r_poolbd])
          wl = w_in[l]
          for hh in range(4):
              DMA("pool", wgp[:, :, C_Q + 64 * hh:C_Q + 64 * hh + 48],
                  wl[:, O_GQ + 48 * hh:O_GQ + 48 * hh + 48].rearrange(KC, p=128), [], [r_wgp])
              DMA("pool", wgp[:, :, C_K + 64 * hh:C_K + 64 * hh + 48],
                  wl[:, O_GK + 48 * hh:O_GK + 48 * hh + 48].rearrange(KC, p=128), [], [r_wgp])
              DMA("pool", wlr2p[0:16, 64 * hh:64 * hh + 48], w_lr2[l, :, 48 * hh:48 * hh + 48], [], [r_wl])
              pr, sl = hh // 2, 64 * (hh % 2)
              DMA("sp", negb[sl:sl + 48, pr:pr + 1], b_lr[l, 48 * hh:48 * hh + 48].rearrange("(p o) -> p o", o=1), [], [r_negb])
              DMA("sp", gngbc[:, hh, :], gng[l].partition_broadcast(128), [], [r_gng])
              DMA("pool", poolbd[sl:sl + 64, pr, sl:sl + 64], pool_w[l, hh], [], [r_poolbd])
          DMA("pool", wgp[:, :, C_V:C_V + 768], wl[:, O_GV:O_GV + 768].rearrange(KC, p=128), [], [r_wgp])
          DMA("pool", wgp[:, :, C_LR:C_LR + 16], wl[:, O_LR:O_LR + 16].rearrange(KC, p=128), [], [r_wgp])
          DMA("pool", wgp[:, :, C_PU:C_PU + 256], wl[:, O_PU:O_PU + 256].rearrange(KC, p=128), [], [r_wgp])
          DMA("pool", wout_gp[:, 0:3, :], w_out[l, 0:384, :].rearrange(KC, p=128), [], [r_wout_gp])
          DMA("pool", wout_gp[:, 3:5, :], w_out[l, 768:1024, :].rearrange(KC, p=128), [], [r_wout_gp])
          for j in range(2):
              DMA("sp", pscol[:, j:j + 1], pool_s[l, 128 * j:128 * j + 128].rearrange("(p o) -> p o", o=1), [], [r_pscol])
          TS("dve", negb, negb, -1.0, None, ALU.mult, None, [r_negb], [r_negb])
          stage_gate(1.2)

          S32, r_S32 = A.alloc("S32", [2, 96], F32)
          Sbf = [A.alloc(f"Sbf{i}", [2, 96], BF16) for i in range(2)]
          stmp, r_stmp = A.alloc("stmp", [2, 96], F32)
          if X0:
              MSET("dve", S32, 0.0, [r_S32])
          else:
              DMA("sp", S32, xs[l], [r_os], [r_S32])
              TS("dve", S32, S32, flag[:, 0:1], None, ALU.mult, None, [r_S32, r_const], [r_S32])
          CP("dve", Sbf[0][0], S32, [r_S32], [Sbf[0][1]])
          sb_i = 0
          ubuf, r_ubuf = A.alloc("ubuf", [2, 528], F32)
          if X0:
              MSET("dve", ubuf[:, :, 0:16], 0.0, [r_ubuf])
          else:
              DMA("sp", ubuf[:, :, 0:16], xu[l], [r_ou], [r_ubuf])
              TS("dve", ubuf[:, :, 0:16], ubuf[:, :, 0:16], flag[:, 0:1], None, ALU.mult, None, [r_ubuf, r_const], [r_ubuf])
          s2, r_s2 = A.alloc("s2", [2, 528], F32)
          s4, r_s4 = A.alloc("s4", [2, 528], F32)
          s8, r_s8 = A.alloc("s8", [528], F32)
          s16, r_s16 = A.alloc("s16", [528], F32)
          pooled, r_pooled = A.alloc("pooled", [2, 512], BF16)
          ptmp, r_ptmp = A.alloc("ptmp", [2, 16], F32)
          lrT, r_lrT = A.alloc("lrT", [512], BF16)
          ta, r_ta = A.alloc("ta", [512], F32)
          cbt, r_cb = A.alloc("cb", [512], F32)
          enb, r_enb = A.alloc("enb", [512], F32)
          eb = [A.alloc(f"eb{j}", [512], F32) for j in range(2)]
          qd, r_qd = A.alloc("qd", [512], BF16)
          qde = [A.alloc(f"qde{j}", [512], BF16) for j in range(2)]
          qdo = [A.alloc(f"qdo{j}", [512], BF16) for j in range(2)]
          kin = [A.alloc(f"kin{j}", [512], BF16) for j in range(2)]
          kintok, r_kintok = A.alloc("kintok", [4, 2, 128], BF16)
          vtok, r_vtok = A.alloc("vtok", [4, 384], BF16)
          gsil, r_gsil = A.alloc("gsil", [4, 384], BF16)
          attm, r_attm = A.alloc("attm", [4, 128], BF16)
          osb, r_osb = A.alloc("osb", [4, 96], F32)
          osq, r_osq = A.alloc("osq", [4, 96], F32)
          oss, r_oss = A.alloc("oss", [4], F32)
          orstd, r_orstd = A.alloc("orstd", [4], F32)
          ogla, r_ogla = A.alloc("ogla", [384], BF16)
          mixgp, r_mixgp = A.alloc("mixgp", [5, 512], BF16)

          for g in range(NG):
              gs = slice(512 * g, 512 * g + 512)
              rA = [ractT[g]]
              for c in range(8):
                  MM(pbf(0)[0:16, :], wgp[:, c, C_LR:C_LR + 16], actT[:, c, gs], c == 0, c == 7, rA + [r_wgp], [rpb[0]])
              CP("act", lrT[0:16, :], pbf(0)[0:16, :], [rpb[0]], [r_lrT])
              stage_gate(1.3)
              for j in range(2):
                  MM(pbf(1), wlr2p[0:16, 128 * j:128 * j + 128], lrT[0:16, :], True, True, [r_wl, r_lrT], [rpb[1]])
                  ACT(ta, pbf(1), AF.Exp, [rpb[1], r_negb], [r_ta], scale=-1.0, bias=negb[:, j:j + 1])
                  ACT(ta, ta, AF.Ln, [r_ta], [r_ta], bias=1.0)
                  P.op("dve", lambda e: e.tensor_tensor_scan(out=cbt, data0=scanmask, data1=ta, initial=0.0,
                                                            op0=ALU.mult, op1=ALU.add), [r_ta, r_c2], [r_cb])
                  ACT(eb[j][0], cbt, AF.Exp, [r_cb], [eb[j][1]], scale=-1.0 / 16)
                  ACT(enb, cbt, AF.Exp, [r_cb], [r_enb], scale=1.0 / 16)
                  if not partial:
                      for c in range(8):
                          MM(pbf(3), wgp[:, c, C_Q + 128 * j:C_Q + 128 * j + 128], actT[:, c, gs], c == 0, c == 7, rA + [r_wgp], [rpb[3]])
                      STT(qd, pbf(3), 48 ** -0.5, eb[j][0], ALU.mult, ALU.mult, [rpb[3], eb[j][1]], [r_qd])
                      TT("pool", qde[j][0], qd, evenmask, ALU.mult, [r_qd, r_c2], [qde[j][1]])
                      TT("pool", qdo[j][0], qd, oddmask, ALU.mult, [r_qd, r_c2], [qdo[j][1]])
                  for c in range(8):
                      MM(pbf(4), wgp[:, c, C_K + 128 * j:C_K + 128 * j + 128], actT[:, c, gs], c == 0, c == 7, rA + [r_wgp], [rpb[4]])
                  TT("dve", kin[j][0], pbf(4), enb, ALU.mult, [rpb[4], r_enb], [kin[j][1]])
              stage_gate(1.4)
              kv_ps = pbb(6).rearrange("p (t j k) -> p t j k", t=4, j=2)
              for tt in range(4):
                  for j in range(2):
                      TR(kv_ps[:, tt, j, :], kin[j][0][:, 128 * tt:128 * tt + 128], [kin[j][1]], [rpb[6]])
              CP("act", kintok, kv_ps, [rpb[6]], [r_kintok])
              stage_gate(1.5)
              for tt in range(4):
                  ts_ = slice(512 * g + 128 * tt, 512 * g + 128 * tt + 128)
                  for c in range(8):
                      MM(pbf(5)[:, 0:384], actT[:, c, ts_], wgp[:, c, C_V:C_V + 384], c == 0, c == 7, rA + [r_wgp], [rpb[5]])
                  CP("act", vtok[:, tt, :], pbf(5)[:, 0:384], [rpb[5]], [r_vtok])
                  if partial:
                      continue
                  for c in range(8):
                      MM(pbf(2)[:, 0:384], actT[:, c, ts_], wgp[:, c, C_G:C_G + 384], c == 0, c == 7, rA + [r_wgp], [rpb[2]])
                  ACT(gsil[:, tt, :], pbf(2)[:, 0:384], AF.Silu, [rpb[2]], [r_gsil])
                  TT("pool", gsil[:, tt, :], gsil[:, tt, :], gngbc.rearrange("p a b -> p (a b)"), ALU.mult, [r_gsil, r_gng], [r_gsil])
              stage_gate(1.6)
              for j in range(2):
                  if partial and g < NG - 1:
                      continue
                  for c in range(8):
                      MM(pbf(1 + j), wgp[:, c, C_PU + 128 * j:C_PU + 128 * j + 128], actT[:, c, gs], c == 0, c == 7,
                         rA + [r_wgp], [rpb[1 + j]])
                  CP("act", ubuf[:, j, 16:528], pbf(1 + j), [rpb[1 + j]], [r_ubuf])
              if not partial:
                  stage_gate(1.61)
                  TT("dve", s2[:, :, 1:528], ubuf[:, :, 1:528], ubuf[:, :, 0:527], ALU.add, [r_ubuf], [r_s2])
                  TT("dve", s4[:, :, 3:528], s2[:, :, 3:528], s2[:, :, 1:526], ALU.add, [r_s2], [r_s4])
                  TT("dve", s8[:, 7:528], s4[:, 1, 7:528], s4[:, 1, 3:524], ALU.add, [r_s4], [r_s8])
                  TT("dve", s16[:, 15:528], s8[:, 15:528], s8[:, 7:520], ALU.add, [r_s8], [r_s16])
                  stage_gate(1.62)
                  for (plo, phi, j, src, rs) in ((0, 64, 0, s2[0:64, 0, 16:528], r_s2), (64, 128, 0, s4[64:128, 0, 16:528], r_s4),
                                                 (0, 64, 1, s8[0:64, 16:528], r_s8), (64, 128, 1, s16[64:128, 16:528], r_s16)):
                      STT(pooled[plo:phi, j, :], src, invw[plo:phi, j:j + 1], ubuf[plo:phi, j, 16:528], ALU.mult, ALU.subtract,
                          [rs, r_ubuf, r_const], [r_pooled])
                      if g == 0:
                          src16 = src[:, 0:16]
                          TT("dve", ptmp[plo:phi, j, :], src16, (invcnt0 if X0 else invcnt)[plo:phi, j, :], ALU.mult, [rs, r_const], [r_ptmp])
                          TT("dve", pooled[plo:phi, j, 0:16], ptmp[plo:phi, j, :], ubuf[plo:phi, j, 16:32], ALU.subtract,
                             [r_ptmp, r_ubuf], [r_pooled])
                  stage_gate(1.63)
                  for j in range(2):
                      MM(pbf(1 + j), poolbd[:, j, :], pooled[:, j, :], True, True, [r_poolbd, r_pooled], [rpb[1 + j]])
                      ACT(mixgp[:, 3 + j, :], pbf(1 + j), AF.Identity, [rpb[1 + j], r_pscol], [r_mixgp], scale=pscol[:, j:j + 1])
              stage_gate(1.64)
              if g == NG - 1:
                  if X0:
                      DMA("sp", oud[l], ubuf[:, :, 512:528], [r_ubuf], [r_ou])
              elif not partial:
                  for j in range(2):
                      CP("act", ubuf[:, j, 0:16], ubuf[:, j, 512:528], [r_ubuf], [r_ubuf])
              stage_gate(1.7)
              pendA = [None]; pendB = [None]
              for tt in range(4):
                  tsl = slice(128 * tt, 128 * tt + 128)
                  at_ps = pbf(0).rearrange("p (a b) -> p a b", a=4)
                  for hh in range(4):
                      if partial:
                          break
                      j, sl = hh // 2, 64 * (hh % 2)
                      MM(at_ps[:, hh, :], kin[j][0][sl:sl + 64, tsl], qde[j][0][sl:sl + 64, tsl], True, False,
                         [kin[j][1], qde[j][1]], [rpb[0]])
                      MM(at_ps[:, hh, :], kin[j][0][sl:sl + 64, tsl], qdo[j][0][sl:sl + 64, tsl], False, True,
                         [kin[j][1], qdo[j][1]], [rpb[0]])
                  if not partial:
                      TT("dve", attm.rearrange("p a b -> p (a b)"), pbf(0), glamask, ALU.mult, [rpb[0], r_c2], [r_attm])
                  if pendA[0] is not None:
                      pendA[0](); pendA[0] = None
                  o_ps = pbf(3)[:, 0:384].rearrange("p (a b) -> p a b", a=4)
                  o2_ps = pbf(5)[:, 0:384].rearrange("p (a b) -> p a b", a=4)
                  for par in range(2):
                      ch = 8 * g + 2 * tt + par
                      rows = slice(64 * par, 64 * par + 64)
                      Scur, r_Scur = Sbf[sb_i]
                      Snxt, r_Snxt = Sbf[1 - sb_i]
                      qsrc = qde if par == 0 else qdo
                      for hh in range(4):
                          if partial:
                              break
                          j, sl = hh // 2, 64 * (hh % 2)
                          if par == 0:
                              MM(o_ps[:, hh, :], attm[:, hh, :], vtok[:, tt, 96 * hh:96 * hh + 96], True, False,
                                 [r_attm, r_vtok], [rpb[3]])
                              MM(o_ps[:, hh, :], qsrc[j][0][sl:sl + 64, tsl], Scur[sl:sl + 64, j, :], False, True,
                                 [qsrc[j][1], r_Scur], [rpb[3]])
                          else:
                              MM(o2_ps[:, hh, :], qsrc[j][0][sl:sl + 64, tsl], Scur[sl:sl + 64, j, :], True, True,
                                 [qsrc[j][1], r_Scur], [rpb[5]])
                      kv_acc = pbf(4)[:, 0:192].rearrange("p (a b) -> p a b", a=2)
                      for hh in range(4):
                          j, sl = hh // 2, 64 * (hh % 2)
                          MM(kv_acc[sl:sl + 64, j, :], kintok[rows, tt, j, sl:sl + 64], vtok[rows, tt, 96 * hh:96 * hh + 96],
                             True, True, [r_kintok, r_vtok], [rpb[4]])
                      TT("dve", stmp, S32, kv_acc, ALU.add, [r_S32, rpb[4]], [r_stmp])
                      col = 64 * (2 * tt + par) + 63
                      for j in range(2):
                          TS("dve", S32[:, j, :], stmp[:, j, :], eb[j][0][:, col:col + 1], None, ALU.mult, None,
                             [r_stmp, eb[j][1]], [r_S32])
                          if not partial:
                              ACT(Snxt[:, j, :], stmp[:, j, :], AF.Identity, [r_stmp, eb[j][1]], [r_Snxt], scale=eb[j][0][:, col:col + 1])
                      sb_i = 1 - sb_i
                  if partial:
                      continue
                  if pendB[0] is not None:
                      pendB[0](); pendB[0] = None

                  def postA(tt=tt, o_ps=o_ps, o2_ps=o2_ps):
                      CP("act", osb, o_ps, [rpb[3]], [r_osb])
                      TT("dve", osb, osb, o2_ps, ALU.add, [r_osb, rpb[5]], [r_osb])
                      TT("dve", osq, osb, osb, ALU.mult, [r_osb], [r_osq])
                      RED(oss, osq, ALU.add, [r_osq], [r_oss])
                      ACT(orstd, oss, AF.Sqrt, [r_oss], [r_orstd], scale=1.0 / 96, bias=EPS)
                      RECIP(orstd, orstd, [r_orstd], [r_orstd])
                      for hh in range(4):
                          STT(ogla[:, 96 * hh:96 * hh + 96], osb[:, hh, :], orstd[:, hh:hh + 1], gsil[:, tt, 96 * hh:96 * hh + 96],
                              ALU.mult, ALU.mult, [r_osb, r_orstd, r_gsil], [r_ogla])

                  def postB(tsl=tsl):
                      og_ps = pbb(7).rearrange("p (c k) -> p c k", c=8)
                      for c in range(3):
                          TR(og_ps[:, c, :], ogla[:, 128 * c:128 * c + 128], [r_ogla], [rpb[7]])
                      CP("act", mixgp[:, 0:3, tsl], og_ps[:, 0:3, :], [rpb[7]], [r_mixgp])

                  pendA[0] = postA
                  pendB[0] = postB
              if pendA[0] is not None:
                  pendA[0](); pendA[0] = None
              if pendB[0] is not None:
                  pendB[0](); pendB[0] = None
              stage_gate(1.8)
              for tt in range(4):
                  if partial:
                      break
                  t = 4 * g + tt
                  for half in range(2):
                      pi = 1 + half
                      for c in range(5):
                          MM(pbf(pi), mixgp[:, c, 128 * tt:128 * tt + 128], wout_gp[:, c, 512 * half:512 * half + 512],
                             c == 0, c == 4, [r_mixgp, r_wout_gp], [rpb[pi]])
                      add_to_h(t, half, pbf(pi), rpb[pi])
              stage_gate(1.9 + 0.01 * g)
          stage_gate(1.95)
          if X0:
              DMA("sp", osd[l], S32, [r_S32], [r_os])
          P.fence(); A.release(m)
          if stages < 3:
              raise _Stop()

          m = A.mark()
          wsb, r_wsb = A.alloc("wsb", [8, 1152], BF16)
          wout_sb, r_wout_sb = A.alloc("wout_sb", [3, D], BF16)
          kT, r_kTprev = A.alloc("kT", [3, 2 * T], BF16)
          r_kTown = P.regs("kTown", NG)
          vall, r_vprev = A.alloc("vall", [32, 384], BF16)
          r_vown = P.regs("vown", NG)
          qT, _ = A.alloc("qT", [3, T], BF16)
          r_qT = P.regs("qT", NG)
          DMA("pool", wsb, w_in[l][:, O_SQ:O_SQ + 1152].rearrange(KC, p=128), [], [r_wsb])
          DMA("pool", wout_sb, w_out[l, 384:768, :].rearrange(KC, p=128), [], [r_wout_sb])
          if not X0:
              DMA("sp", kT[:, :, 0:T], xk[l], [r_ok], [r_kTprev])
              DMA("sp", vall[:, 0:16, :], xv[l], [r_ov], [r_vprev])
          for g in range(NG):
              gs = slice(512 * g, 512 * g + 512)
              rA = [ractT[g]]
              for pr in range(3):
                  if not partial:
                      for c in range(8):
                          MM(pbf(0), wsb[:, c, 128 * pr:128 * pr + 128], actT[:, c, gs], c == 0, c == 7, rA + [r_wsb], [rpb[0]])
                      ACT(qT[:, pr, gs], pbf(0), AF.Copy, [rpb[0]], [r_qT[g]], scale=0.125)
                  for c in range(8):
                      MM(pbf(1), wsb[:, c, 384 + 128 * pr:384 + 128 * pr + 128], actT[:, c, gs], c == 0, c == 7, rA + [r_wsb], [rpb[1]])
                  CP("dve", kT[:, pr, T + 512 * g:T + 512 * g + 512], pbf(1), [rpb[1]], [r_kTown[g]])
              for tt in range(4):
                  t = 4 * g + tt
                  ts_ = slice(128 * t, 128 * t + 128)
                  for c in range(8):
                      MM(pbf(2)[:, 0:384], actT[:, c, ts_], wsb[:, c, 768:1152], c == 0, c == 7, rA + [r_wsb], [rpb[2]])
                  CP("act", vall[:, 16 + t, :], pbf(2)[:, 0:384], [rpb[2]], [r_vown[g]])
          if X0:
              DMA("sp", ok[l], kT[:, :, T:2 * T], r_kTown, [r_ok])
              DMA("sp", ov[l], vall[:, 16:32, :], r_vown, [r_ov])
          if partial:
              raise _SegEnd()
          e_t = [A.alloc(f"e{i}", [512], F32) for i in range(3)]
          sp_t = [A.alloc(f"sp{i}", [512], BF16) for i in range(2)]
          en_t = [A.alloc(f"en{i}", [512], BF16) for i in range(2)]
          a_t = [A.alloc(f"a{i}", [512], BF16) for i in range(2)]
          spsum_t = [A.alloc(f"spsum{i}", [512], BF16) for i in range(2)]
          osbT, r_osbT = A.alloc("osbT", [3, 512], BF16)
          it = 0
          NFILL = 2
          for g in range(NG):
              gs = slice(512 * g, 512 * g + 512)
              for pr in range(3):
                  o_ps, r_o = pbf(4), rpb[4]
                  for par in range(2):
                      rows = slice(64 * par, 64 * par + 64)
                      hd = 2 * pr + par
                      blocks = [(16 + 4 * g + j, j) for j in (3, 2, 1, 0)] + [(kb, None) for kb in range(16 + 4 * g - 1, (15 if X0 else -1), -1)]
                      MSET("dve", spsum_t[0][0], 0.0, [spsum_t[0][1]])
                      nb = len(blocks)

                      def bufs(bi):
                          i2 = bi % 2
                          return (pbf(i2), rpb[i2], pbf(2 + i2), rpb[2 + i2]) + e_t[bi % 3] + sp_t[i2] + en_t[i2] + a_t[i2]

                      def kv_regs(kb):
                          if kb >= 16:
                              return [r_kTown[(kb - 16) // 4]], [r_vown[(kb - 16) // 4]]
                          return [r_kTprev], [r_vprev]

                      def sb_z(bi):
                          kb, dj = blocks[bi]
                          z_ps, r_z, n_ps, r_n, e, r_e, sp, r_sp, en, r_en, a, r_a = bufs(bi)
                          rk, rv = kv_regs(kb)
                          MM(z_ps, kT[rows, pr, 128 * kb:128 * kb + 128], qT[rows, pr, gs], True, True, rk + [r_qT[g]], [r_z])
                          if kb >= 16:
                              ACT(e, z_ps, AF.Exp, [r_z], [r_e])
                          else:
                              ACT(e, z_ps, AF.Exp, [r_z, r_const], [r_e], bias=prevbias[:, 0:1])
                          if dj is not None:
                              w = 128 * (dj + 1)
                              TT("dve", e[:, 0:w], e[:, 0:w], sbmask[:, 512 - w:512], ALU.mult, [r_e, r_const], [r_e])
                          ACT(sp, e, AF.Ln, [r_e], [r_sp], bias=1.0)

                      def sb_n(bi):
                          z_ps, r_z, n_ps, r_n, e, r_e, sp, r_sp, en, r_en, a, r_a = bufs(bi)
                          cur, r_cur = spsum_t[bi % 2]
                          nxt, r_nxt = spsum_t[(bi + 1) % 2]
                          MM(n_ps, tri, sp, True, bi == 0, [r_const, r_sp], [r_n])
                          if bi > 0:
                              MM(n_ps, ones, cur, False, True, [r_const, r_cur], [r_n])
                          ACT(en, n_ps, AF.Exp, [r_n], [r_en], scale=-1.0)
                          if bi + 1 < nb:
                              TT("dve", nxt, cur, sp, ALU.add, [r_cur, r_sp], [r_nxt])
                          TT("dve", a, e, en, ALU.mult, [r_e, r_en], [r_a])

                      def sb_fill(n):
                          for _ in range(n):
                              MM(pbf(5), ones, sbmask, True, True, [r_const], [rpb[5]])

                      def sb_av(bi):
                          kb, dj = blocks[bi]
                          z_ps, r_z, n_ps, r_n, e, r_e, sp, r_sp, en, r_en, a, r_a = bufs(bi)
                          rk, rv = kv_regs(kb)
                          MM(o_ps[rows, :], vall[:, kb, 64 * hd:64 * hd + 64], a, bi == 0, bi == nb - 1, rv + [r_a], [r_o])

                      sb_z(0)
                      for bi in range(nb):
                          if bi + 1 < nb:
                              sb_z(bi + 1)
                          sb_n(bi)
                          if bi >= 1:
                              sb_fill(NFILL)
                              sb_av(bi - 1)
                      sb_av(nb - 1)
                  CP("act", osbT[:, pr, :], o_ps, [r_o], [r_osbT])
              for tt in range(4):
                  t = 4 * g + tt
                  for half in range(2):
                      pi = 2 + half
                      for c in range(3):
                          MM(pbf(pi), osbT[:, c, 128 * tt:128 * tt + 128], wout_sb[:, c, 512 * half:512 * half + 512],
                             c == 0, c == 2, [r_osbT, r_wout_sb], [rpb[pi]])
                      add_to_h(t, half, pbf(pi), rpb[pi])
          P.fence(); A.release(m)
          if stages < 4:
              raise _Stop()

          m = A.mark()
          norm_to_T(ng[l, 1])
          P.fence(); A.release(m)
          m = A.mark()
          cw, r_cw = A.alloc("cw", [NT, 32], F32)
          wrt, r_wrt = A.alloc("wrt", [8, 36], BF16)
          brbc, r_brbc = A.alloc("brbc", [36], F32)
          DMA("pool", wrt, wr[l].rearrange(KC, p=128), [], [r_wrt])
          DMA("sp", brbc, br[l].partition_broadcast(128), [], [r_brbc])
          NRB = 3
          rt = {}
          for nm, shp in (("lg", [36]), ("gmax", [1]), ("ngmax", [1]), ("gex", [4]), ("gsum", [1]), ("gw", [1]), ("oh", [4]),
                          ("esel", [8]), ("m1", [1]), ("mk1", [8]), ("e2", [8]), ("m2", [1]), ("mk2", [8]), ("dd", [1]),
                          ("w1", [1]), ("w2", [1]), ("cs", [8])):
              rt[nm] = [A.alloc(f"rt_{nm}{i}", shp, F32) for i in range(NRB)]

          def router_a(t):
              ts_ = slice(128 * t, 128 * t + 128)
              pi = t % 2
              for c in range(8):
                  MM(pbf(pi)[:, 0:36], actT[:, c, ts_], wrt[:, c, :], c == 0, c == 7, [ractT[t // 4], r_wrt], [rpb[pi]])
              lg, r_lg = rt["lg"][t % NRB]
              gmax, r_gmax = rt["gmax"][t % NRB]; ngmax, r_ngmax = rt["ngmax"][t % NRB]
              gex, r_gex = rt["gex"][t % NRB]; gsum, r_gsum = rt["gsum"][t % NRB]
              TT("dve", lg, pbf(pi)[:, 0:36], brbc, ALU.add, [rpb[pi], r_brbc], [r_lg])
              RED(gmax, lg[:, 0:4], ALU.max, [r_lg], [r_gmax])
              TS("dve", ngmax, gmax, -1.0, None, ALU.mult, None, [r_gmax], [r_ngmax])
              ACT(gex, lg[:, 0:4], AF.Exp, [r_lg, r_ngmax], [r_gex, r_gsum], bias=ngmax[:, 0:1], accum_out=gsum)

          def router_b(t):
              b_ = t % NRB
              lg, r_lg = rt["lg"][b_]; gmax, r_gmax = rt["gmax"][b_]; gsum, r_gsum = rt["gsum"][b_]
              gw, r_gw = rt["gw"][b_]; oh, r_oh = rt["oh"][b_]; esel, r_esel = rt["esel"][b_]; m1, r_m1 = rt["m1"][b_]
              mk1, r_mk1 = rt["mk1"][b_]; e2, r_e2 = rt["e2"][b_]; m2, r_m2 = rt["m2"][b_]; mk2, r_mk2 = rt["mk2"][b_]
              dd, r_dd = rt["dd"][b_]; w1, r_w1 = rt["w1"][b_]
              TS("dve", oh, lg[:, 0:4], gmax[:, 0:1], None, ALU.is_equal, None, [r_lg, r_gmax], [r_oh])
              TS("dve", esel, lg[:, 4:12], oh[:, 0:1], None, ALU.mult, None, [r_lg, r_oh], [r_esel])
              for gi in range(1, 4):
                  STT(esel, lg[:, 4 + 8 * gi:12 + 8 * gi], oh[:, gi:gi + 1], esel, ALU.mult, ALU.add, [r_lg, r_oh, r_esel], [r_esel])
              RED(m1, esel, ALU.max, [r_esel], [r_m1])
              TS("dve", mk1, esel, m1[:, 0:1], None, ALU.is_equal, None, [r_esel, r_m1], [r_mk1])
              STT(e2, mk1, -1e30, esel, ALU.mult, ALU.add, [r_mk1, r_esel], [r_e2])
              RED(m2, e2, ALU.max, [r_e2], [r_m2])
              TS("dve", mk2, e2, m2[:, 0:1], None, ALU.is_equal, None, [r_e2, r_m2], [r_mk2])
              TT("dve", dd, m1, m2, ALU.subtract, [r_m1, r_m2], [r_dd])
              ACT(w1, dd, AF.Sigmoid, [r_dd], [r_w1])
              RECIP(gw, gsum, [r_gsum], [r_gw])

          def router_c(t):
              b_ = t % NRB
              gw, r_gw = rt["gw"][b_]; oh, r_oh = rt["oh"][b_]; mk1, r_mk1 = rt["mk1"][b_]; mk2, r_mk2 = rt["mk2"][b_]
              w1, r_w1 = rt["w1"][b_]; w2, r_w2 = rt["w2"][b_]; cs, r_cs = rt["cs"][b_]
              TT("dve", w1, w1, gw, ALU.mult, [r_w1, r_gw], [r_w1])
              TT("dve", w2, gw, w1, ALU.subtract, [r_gw, r_w1], [r_w2])
              TS("dve", cs, mk1, w1[:, 0:1], None, ALU.mult, None, [r_mk1, r_w1], [r_cs])
              STT(cs, mk2, w2[:, 0:1], cs, ALU.mult, ALU.add, [r_mk2, r_w2, r_cs], [r_cs])
              for gi in range(4):
                  TS("dve", cw[:, t, 8 * gi:8 * gi + 8], cs, oh[:, gi:gi + 1], None, ALU.mult, None, [r_cs, r_oh], [r_cw])

          for t in range(NT + 2):
              if t < NT:
                  router_a(t)
              if 1 <= t <= NT:
                  router_b(t - 1)
              if t >= 2:
                  router_c(t - 2)
          if stages < 5:
              raise _Stop()

          NBUF = 2
          wgu_b = [A.alloc(f"wgu{i}", [2, 8, 512], BF16) for i in range(NBUF)]
          wd_b = [A.alloc(f"wd{i}", [2, 2, D], BF16) for i in range(NBUF)]
          sg_t = [A.alloc(f"sg{i}", [256], F32) for i in range(2)]
          hid_t = [A.alloc(f"hid{i}", [256], BF16) for i in range(2)]
          hidT_t = [A.alloc(f"hidT{i}", [2, 128], BF16) for i in range(2)]
          steps = [(ep, t, s_) for ep in range(NEXP // 2) for t in range(NT) for s_ in range(2)]

          def moe_ab(k):
              ep, t, s_ = steps[k]
              wgu, r_wgu = wgu_b[ep % NBUF]
              wdd, r_wd = wd_b[ep % NBUF]
              if t == 0 and s_ == 0:
                  for s2_ in range(2):
                      e_ = 2 * ep + s2_
                      DMA("pool", wgu[:, s2_, :, 0:256], wg[l, e_].rearrange(KC, p=128), [], [r_wgu])
                      DMA("pool", wgu[:, s2_, :, 256:512], wu[l, e_].rearrange(KC, p=128), [], [r_wgu])
                      DMA("pool", wdd[:, s2_, :, :], wd[l, e_].rearrange(KC, p=128), [], [r_wd])
              e_ = 2 * ep + s_
              i2 = k % 2
              ts_ = slice(128 * t, 128 * t + 128)
              ab, r_ab = pbf(i2), rpb[i2]
              for c in range(8):
                  MM(ab, actT[:, c, ts_], wgu[:, s_, c, :], c == 0, c == 7, [ractT[t // 4], r_wgu], [r_ab])
              sg, r_sg = sg_t[i2]; hid, r_hid = hid_t[i2]
              ACT(sg, ab[:, 0:256], AF.Silu, [r_ab], [r_sg])
              STT(hid, ab[:, 256:512], cw[:, t, e_:e_ + 1], sg, ALU.mult, ALU.mult, [r_ab, r_cw, r_sg], [r_hid])

          def moe_tr(k):
              i2 = k % 2
              hid, r_hid = hid_t[i2]; hidT, r_hidT = hidT_t[i2]
              tp = pbb(6 + i2).rearrange("p (c k) -> p c k", c=8)
              for f in range(2):
                  TR(tp[:, f, :], hid[:, 128 * f:128 * f + 128], [r_hid], [rpb[6 + i2]])
              CP("act", hidT, tp[:, 0:2, :], [rpb[6 + i2]], [r_hidT])

          def moe_down(k):
              ep, t, s_ = steps[k]
              wdd, r_wd = wd_b[ep % NBUF]
              hidT, r_hidT = hidT_t[k % 2]
              yb = 2 + 2 * (t % 2)
              for half in range(2):
                  for f in range(2):
                      MM(pbf(yb + half), hidT[:, f, :], wdd[:, s_, f, 512 * half:512 * half + 512],
                         s_ == 0 and f == 0, s_ == 1 and f == 1, [r_hidT, r_wd], [rpb[yb + half]])
              if s_ == 1:
                  for half in range(2):
                      add_to_h(t, half, pbf(yb + half), rpb[yb + half])

          nst = len(steps)
          moe_ab(0)
          for k in range(nst):
              if k + 1 < nst:
                  moe_ab(k + 1)
              moe_tr(k)
              if k >= 1:
                  moe_down(k - 1)
          moe_down(nst - 1)
          P.fence(); A.release(m)
          if stages < 6:
              raise _Stop()

          m = A.mark()
          norm_to_T(ng[l, 2])
          P.fence(); A.release(m)
          m = A.mark()
          gwt, r_gwt = A.alloc("gwt", [8, D], BF16)
          pwt, r_pwt = A.alloc("pwt", [2, D], BF16)
          gbbc, r_gbbc = A.alloc("gbbc", [D], F32)
          DMA("pool", gwt, pgw[l].rearrange(KC, p=128), [], [r_gwt])
          DMA("pool", pwt, ppw[l].rearrange(KC, p=128), [], [r_pwt])
          DMA("sp", gbbc, pgb[l].partition_broadcast(128), [], [r_gbbc])
          pt_t = [A.alloc(f"pt{i}", [256], BF16) for i in range(2)]
          pT_t = [A.alloc(f"pT{i}", [2, 128], BF16) for i in range(2)]
          gate_t = [A.alloc(f"gate{i}", [D], F32) for i in range(2)]
          for t in range(NT):
              ts_ = slice(128 * t, 128 * t + 128)
              i2 = t % 2
              pt, r_pt = pt_t[i2]; pT, r_pT = pT_t[i2]; gate, r_gate = gate_t[i2]
              DMA("pool", pt, p_in[l, 128 * t:128 * t + 128, :], [], [r_pt])
              tp = pbb(6 + i2).rearrange("p (c k) -> p c k", c=8)
              for f in range(2):
                  TR(tp[:, f, :], pt[:, 128 * f:128 * f + 128], [r_pt], [rpb[6 + i2]])
              CP("act", pT, tp[:, 0:2, :], [rpb[6 + i2]], [r_pT])
              def ple_gate_mm(half):
                  hs = slice(512 * half, 512 * half + 512)
                  gp, r_gp = pbf(half), rpb[half]
                  for c in range(8):
                      MM(gp, actT[:, c, ts_], gwt[:, c, hs], c == 0, c == 7, [ractT[t // 4], r_gwt], [r_gp])

              def ple_e_mm(half):
                  hs = slice(512 * half, 512 * half + 512)
                  ep_, r_ep = pbf(2 + 2 * i2 + half), rpb[2 + 2 * i2 + half]
                  for f in range(2):
                      MM(ep_, pT[:, f, :], pwt[:, f, hs], f == 0, f == 1, [r_pT, r_pwt], [r_ep])

              ple_gate_mm(0); ple_e_mm(0); ple_e_mm(1); ple_gate_mm(1)
              for half in range(2):
                  hs = slice(512 * half, 512 * half + 512)
                  gp, r_gp = pbf(half), rpb[half]
                  ep_, r_ep = pbf(2 + 2 * i2 + half), rpb[2 + 2 * i2 + half]
                  r_gh = P.reg(f"gate{i2}h{half}")
                  TT("dve", gate[:, hs], gp, gbbc[:, hs], ALU.add, [r_gp, r_gbbc], [r_gh])
                  ACT(gate[:, hs], gate[:, hs], AF.Sigmoid, [r_gh], [r_gh])
                  TT("dve", gate[:, hs], gate[:, hs], ep_, ALU.mult, [r_gh, r_ep], [r_gh])
                  TT("dve", h[:, t, hs], h[:, t, hs], gate[:, hs], ALU.add, [rh[t], r_gh], [rh[t]])
          P.fence(); A.release(m)
      except _SegEnd:
        P.fence(); A.release(m_layer)
      except _Stop:
        P.fence(); A.release(m_layer)
        stopped = True

    m = A.mark()
    if stages <= 0:
        P.emit(outputs_final=[])
        es.close()
        return nc
    gbc, r_g = A.alloc("fgbc", [D], F32)
    junk, r_junk = A.alloc("fjunk", [D], BF16)
    ssq, r_ss = A.alloc("fssq", [NT], F32)
    rstd, r_rstd = A.alloc("frstd", [NT], F32)
    yt = [A.alloc(f"yt{i}", [D], F32) for i in range(2)]
    DMA("sp", gbc, fg.partition_broadcast(128), [], [r_g])
    for t in range(NT):
        ACT(junk, h[:, t, :], AF.Square, [rh[t]], [r_junk, r_ss], accum_out=ssq[:, t:t + 1])
    ACT(rstd, ssq, AF.Sqrt, [r_ss], [r_rstd], scale=1.0 / D, bias=EPS)
    RECIP(rstd, rstd, [r_rstd], [r_rstd])
    for t in range(NT):
        yb_, r_yb = yt[t % 2]
        STT(yb_, h[:, t, :], rstd[:, t:t + 1], gbc, ALU.mult, ALU.mult, [rh[t], r_rstd, r_g], [r_yb])
        DMA("sp", y_out[128 * t:128 * t + 128, :], yb_, [r_yb], [r_yout])
    P.emit(outputs_final=[r_yout])
    print("arena high-water KiB:", A.hi / 1024, "ops:", P.n, flush=True)
    es.close()
    return nc


def _consts():
    c = {}
    c["c_ident"] = np.eye(128, dtype=np.float32).astype(NPBF)
    jj = np.arange(128)
    c["c_tri"] = (jj[:, None] >= jj[None, :]).astype(np.float32).astype(NPBF)
    c["c_ones"] = np.ones((128, 128), np.float32).astype(NPBF)
    sb = np.zeros((128, 512), np.float32)
    sb[:, 384:512] = (jj[:, None] < jj[None, :])
    c["c_sbmask"] = sb.astype(NPBF)
    blk = ((jj[:, None] // 64) == (jj[None, :] // 64)) & (jj[:, None] <= jj[None, :])
    c["c_glamask"] = np.tile(blk.astype(np.float32), (1, 4)).astype(NPBF)
    col = np.arange(512)
    c["c_scanmask"] = np.tile((col % 64 != 0).astype(np.float32)[None, :], (128, 1)).astype(NPBF)
    ev = ((col // 64) % 2 == 0).astype(np.float32)
    c["c_evenmask"] = np.tile(ev[None, :], (128, 1)).astype(NPBF)
    c["c_oddmask"] = np.tile((1 - ev)[None, :], (128, 1)).astype(NPBF)
    invw = np.zeros((128, 2), np.float32)
    invw[0:64, 0] = 1 / 2; invw[64:, 0] = 1 / 4; invw[0:64, 1] = 1 / 8; invw[64:, 1] = 1 / 16
    c["c_invw"] = invw
    c["c_invcnt0"] = _role(0)["r_invcnt"]
    return c


def _role(r):
    d = {}
    d["r_prevbias"] = np.full((128, 1), 0.0 if r == 1 else -30000.0, np.float32)
    d["r_flag"] = np.full((128, 1), float(r), np.float32)
    w = np.zeros((128, 2), np.float32)
    w[0:64, 0] = 2; w[64:, 0] = 4; w[0:64, 1] = 8; w[64:, 1] = 16
    t = np.arange(16, dtype=np.float32)
    if r == 0:
        cnt = np.minimum(t[None, None, :] + 1.0, w[:, :, None])
    else:
        cnt = np.broadcast_to(w[:, :, None], (128, 2, 16))
    d["r_invcnt"] = (1.0 / cnt).astype(np.float32)
    return d


def _layer_weights(inp, ls):
    f = lambda k: np.ascontiguousarray(inp[k][ls])
    d = {}
    d["ng"] = np.ascontiguousarray(np.stack([inp["norm1_g"][ls], inp["norm2_g"][ls], inp["ple_norm_g"][ls]], axis=1))
    d["fg"] = np.ascontiguousarray(inp["final_norm_g"])
    d["w_in"] = f("w_in"); d["w_lr2"] = f("gla_w_lr2"); d["b_lr"] = f("gla_b_lr"); d["gng"] = f("gla_norm_g")
    d["pool_w"] = f("pool_w"); d["pool_s"] = f("pool_scale"); d["w_out"] = f("w_out")
    d["wr"] = np.ascontiguousarray(np.concatenate([inp["router_group_w"][ls], inp["router_exp_w"][ls]], axis=-1))
    d["br"] = np.ascontiguousarray(np.concatenate([inp["router_group_b"][ls], inp["router_exp_b"][ls]], axis=-1))
    d["wg"] = f("exp_w_gate"); d["wu"] = f("exp_w_up"); d["wd"] = f("exp_w_down")
    d["pgw"] = f("ple_gate_w"); d["pgb"] = f("ple_gate_b"); d["ppw"] = f("ple_proj_w")
    return d


_NC_CACHE = {}


def _get_nc(L, stages=99):
    key = (L, stages)
    if key not in _NC_CACHE:
        _NC_CACHE[key] = build(L, stages)
    return _NC_CACHE[key]


def make_maps(inp, L=DEPTH, batches=(0, 1, 2, 3), roles=(0, 1)):
    consts = _consts()
    lw = _layer_weights(inp, slice(0, L))
    maps = []
    for b in batches:
        for r in roles:
            m = dict(consts); m.update(lw); m.update(_role(r))
            m["h0a"] = np.ascontiguousarray(inp["x"][b, 0:T])
            m["pa"] = np.ascontiguousarray(inp["p"][0:L, b, 0:T, :])
            m["h0b"] = np.ascontiguousarray(inp["x"][b, r * T:(r + 1) * T])
            m["pb"] = np.ascontiguousarray(inp["p"][0:L, b, r * T:(r + 1) * T, :])
            maps.append(m)
    return maps


def kernel(**inputs):
    inp = {k: np.asarray(v) for k, v in inputs.items()}
    B = inp["x"].shape[0]
    nc = _get_nc(DEPTH)
    maps = make_maps(inp, DEPTH, tuple(range(B)), (0, 1))
    res = run_bass_kernel_spmd(nc, maps, core_ids=list(range(len(maps))))
    out = np.zeros((B, 2 * T, D), np.float32)
    i = 0
    for b in range(B):
        for r in (0, 1):
            out[b, r * T:(r + 1) * T] = res.results[i]["y_out"]
            i += 1
    return out
```
